# Optimizing a Trainium2 kernel written in Bass

```python
import jax
import jax.numpy as jnp
from jax import lax
import numpy as np

D_MODEL = 2048
BATCH = 8
SEQ = 4096
DEPTH = 4

CTX_LEN = 256
GRID_W = 64
EPS = 1e-6
N_MOD = 6

LRU_WIDTH = 512
LRU_BLOCKS = 4
LRU_BLOCK = LRU_WIDTH // LRU_BLOCKS
CONV_W = 4
LRU_C = 8.0
A_MIN = 0.9
A_MAX = 0.999
HG_HEADS = 4
HG_DK = 128
HG_DV = 128
HG_WIDTH = HG_HEADS * HG_DK
HG_CHUNK = 64
HEAD_DIM = 128
N_Q_HEADS = 8
N_KV_HEADS = 2
Q_PER_KV = N_Q_HEADS // N_KV_HEADS
ATT_WIDTH = N_Q_HEADS * HEAD_DIM
KV_WIDTH = N_KV_HEADS * HEAD_DIM
Q_BLOCK = 128
ROPE_THETA = 10000.0
ROPE_FREQS = HEAD_DIM // 4
ATTN_SCALE = HEAD_DIM ** -0.5
A_IN = 2 * LRU_WIDTH
B_IN = 5 * HG_WIDTH
C_IN = ATT_WIDTH + 2 * KV_WIDTH
IN_WIDTH = A_IN + B_IN + C_IN
MIX_WIDTH = LRU_WIDTH + HG_HEADS * HG_DV + ATT_WIDTH
D_FF = 5632
N_EXPERTS = 8
TOP_K = 2
N_DENSE = (DEPTH + 1) // 2
N_MOE = DEPTH // 2

kernel_name = 'hybrid_rglru_hgrn2_gqa_moe_dit'


def rmsnorm(x, g):
    xf = x.astype(jnp.float32)
    y = xf * lax.rsqrt(jnp.mean(xf * xf, axis=-1, keepdims=True) + EPS)
    return (y * g.astype(jnp.float32)).astype(x.dtype)


def modulate(h, mod, i):
    return h * (1 + mod[:, :, i + 1]) + mod[:, :, i]


def _flip(t):
    return jnp.flip(t, axis=1)


def _ident(t):
    return t


def rope_tables(rows):
    row = jnp.repeat(jnp.arange(rows, dtype=jnp.float32), GRID_W)
    col = jnp.tile(jnp.arange(GRID_W, dtype=jnp.float32), rows)
    inv = ROPE_THETA ** (-jnp.arange(ROPE_FREQS, dtype=jnp.float32) / ROPE_FREQS)
    ang = jnp.stack([row[:, None] * inv, col[:, None] * inv], axis=1)
    return jnp.cos(ang), jnp.sin(ang)


def apply_rope_2d(x, cos, sin):
    xr = x.astype(jnp.float32).reshape(*x.shape[:-1], 2, 2, ROPE_FREQS)
    x1, x2 = xr[..., 0, :], xr[..., 1, :]
    cs, sn = cos[None, :, None], sin[None, :, None]
    out = jnp.stack([x1 * cs - x2 * sn, x2 * cs + x1 * sn], axis=-2)
    return out.reshape(x.shape).astype(x.dtype)


def dwconv_centred(x, w, b):
    y = lax.conv_general_dilated(
        x, w[:, None, :], window_strides=(1,),
        padding=[(CONV_W // 2, CONV_W - 1 - CONV_W // 2)],
        dimension_numbers=('NWC', 'WIO', 'NWC'), feature_group_count=x.shape[-1])
    return y + b


def block_diag(u, w, b):
    ub = u.reshape(*u.shape[:-1], LRU_BLOCKS, LRU_BLOCK)
    return jnp.einsum('btnk,nkj->btnj', ub, w).reshape(u.shape) + b


def _lin_combine(l, r):
    al, bl = l
    ar, br = r
    return al * ar, ar * bl + br


def rglru_scan(u, wa, ba, wi, bi, lam, h0):
    r = jax.nn.sigmoid(block_diag(u, wa, ba))
    i = jax.nn.sigmoid(block_diag(u, wi, bi))
    log_a = -LRU_C * r * jax.nn.softplus(-lam)
    a = jnp.exp(log_a)
    bt = jnp.sqrt(-jnp.expm1(2.0 * log_a)) * (i * u)
    bt = bt.at[:, 0].add(a[:, 0] * h0)
    _, h = lax.associative_scan(_lin_combine, (a, bt), axis=1)
    return h


def mixer_rglru(ua_ctx, ua_lat, conv_w, conv_b, wa, ba, wi, bi, lam, need_ctx):
    y_c, x_c = jnp.split(ua_ctx, 2, axis=-1)
    y_l, x_l = jnp.split(ua_lat, 2, axis=-1)
    x_c = dwconv_centred(x_c, conv_w, conv_b).astype(jnp.float32)
    x_l = dwconv_centred(x_l, conv_w, conv_b).astype(jnp.float32)
    h_c, h_l = [], []
    for d in range(2):
        fl = _flip if d else _ident
        hc = rglru_scan(fl(x_c), wa[d], ba[d], wi[d], bi[d], lam[d], jnp.zeros_like(x_c[:, 0]))
        hl = rglru_scan(fl(x_l), wa[d], ba[d], wi[d], bi[d], lam[d], hc[:, -1])
        h_c.append(fl(hc))
        h_l.append(fl(hl))
    out_l = (jax.nn.gelu(y_l.astype(jnp.float32)) * (h_l[0] + h_l[1])).astype(ua_lat.dtype)
    out_c = None
    if need_ctx:
        out_c = (jax.nn.gelu(y_c.astype(jnp.float32)) * (h_c[0] + h_c[1])).astype(ua_ctx.dtype)
    return out_c, out_l


def hgrn_chunk_scan(q, k, v, g, s0):
    bsz, t = q.shape[:2]
    nch = t // HG_CHUNK

    def to_chunks(z):
        return z.reshape(bsz, nch, HG_CHUNK, HG_HEADS, z.shape[-1]).transpose(1, 0, 3, 2, 4)

    mask = jnp.tril(jnp.ones((HG_CHUNK, HG_CHUNK), dtype=bool))

    def step(s, inp):
        qc, kc, vc, gc = inp
        bc = jnp.cumsum(gc, axis=2)
        o_inter = jnp.einsum('bhtk,bhkv->bhtv', qc * jnp.exp(bc), s)
        diff = bc[:, :, :, None, :] - bc[:, :, None, :, :]
        decay = jnp.exp(jnp.where(mask[:, :, None], diff, -jnp.inf))
        att = jnp.einsum('bhtk,bhsk,bhtsk->bhts', qc, kc, decay)
        o = o_inter + jnp.einsum('bhts,bhsv->bhtv', att, vc)
        blast = bc[:, :, -1:, :]
        s = jnp.exp(blast[:, :, 0, :])[..., None] * s + jnp.einsum(
            'bhsk,bhsv->bhkv', kc * jnp.exp(blast - bc), vc)
        return s, o

    s, o = lax.scan(step, s0, (to_chunks(q), to_chunks(k), to_chunks(v), to_chunks(g)))
    o = o.transpose(1, 0, 3, 2, 4).reshape(bsz, t, HG_HEADS, HG_DV)
    return o, s


def mixer_hgrn(ub_ctx, ub_lat, lb_f, lb_b, gnorm, need_ctx):
    def prep(u):
        uf = u.astype(jnp.float32)
        q, ff, fb, i, g = jnp.split(uf, 5, axis=-1)
        hd = lambda z: z.reshape(z.shape[0], z.shape[1], HG_HEADS, HG_DK)
        return hd(jax.nn.silu(q)), hd(ff), hd(fb), hd(i), g

    def forget(f_logit, lb):
        f = lb + (1 - lb) * jax.nn.sigmoid(f_logit)
        return 1 - f, jnp.log(f)

    q_c, ff_c, fb_c, i_c, og_c = prep(ub_ctx)
    q_l, ff_l, fb_l, i_l, og_l = prep(ub_lat)
    bsz = ub_lat.shape[0]
    o_c, o_l = [], []
    for d, (lb, f_c, f_l) in enumerate(((lb_f, ff_c, ff_l), (lb_b, fb_c, fb_l))):
        fl = _flip if d else _ident
        lbh = lb.astype(jnp.float32).reshape(HG_HEADS, HG_DK)
        k_c, lg_c = forget(f_c, lbh)
        k_l, lg_l = forget(f_l, lbh)
        s0 = jnp.zeros((bsz, HG_HEADS, HG_DK, HG_DV), jnp.float32)
        oc, sc = hgrn_chunk_scan(fl(q_c), fl(k_c), fl(i_c), fl(lg_c), s0)
        ol, _ = hgrn_chunk_scan(fl(q_l), fl(k_l), fl(i_l), fl(lg_l), sc)
        o_c.append(fl(oc))
        o_l.append(fl(ol))

    def readout(o, og, dt):
        y = rmsnorm(o, gnorm).reshape(og.shape) * jax.nn.silu(og)
        return y.astype(dt)

    out_l = readout(o_l[0] + o_l[1], og_l, ub_lat.dtype)
    out_c = readout(o_c[0] + o_c[1], og_c, ub_ctx.dtype) if need_ctx else None
    return out_c, out_l


def gqa_attend(q, k, v):
    bsz, tq = q.shape[:2]
    qg = q.reshape(bsz, tq, N_KV_HEADS, Q_PER_KV, HEAD_DIM)
    s = jnp.einsum('bqngd,bknd->bngqk', qg, k).astype(jnp.float32) * ATTN_SCALE
    p = jax.nn.softmax(s, axis=-1).astype(v.dtype)
    o = jnp.einsum('bngqk,bknd->bqngd', p, v)
    return o.reshape(bsz, tq, ATT_WIDTH)


def mixer_attn(uc_ctx, uc_lat, qn, kn, cos, sin, need_ctx):
    def qkv(u):
        bsz, t = u.shape[:2]
        q, k, v = jnp.split(u, [ATT_WIDTH, ATT_WIDTH + KV_WIDTH], axis=-1)
        q = rmsnorm(q.reshape(bsz, t, N_Q_HEADS, HEAD_DIM), qn)
        k = rmsnorm(k.reshape(bsz, t, N_KV_HEADS, HEAD_DIM), kn)
        return q, k, v.reshape(bsz, t, N_KV_HEADS, HEAD_DIM)

    q_c, k_c, v_c = qkv(uc_ctx)
    q_l, k_l, v_l = qkv(uc_lat)
    q_l = apply_rope_2d(q_l, cos, sin)
    k_l = apply_rope_2d(k_l, cos, sin)
    k_all = jnp.concatenate([k_c, k_l], axis=1)
    v_all = jnp.concatenate([v_c, v_l], axis=1)
    bsz, n = q_l.shape[:2]
    nblk = n // Q_BLOCK
    qb = q_l.reshape(bsz, nblk, Q_BLOCK, N_Q_HEADS, HEAD_DIM).swapaxes(0, 1)
    o_l = lax.map(lambda blk: gqa_attend(blk, k_all, v_all), qb)
    o_l = o_l.swapaxes(0, 1).reshape(bsz, n, ATT_WIDTH)
    o_c = gqa_attend(q_c, k_c, v_c) if need_ctx else None
    return o_c, o_l


def swiglu(h, w1, w3, w2):
    return (jax.nn.silu(h @ w1) * (h @ w3)) @ w2


def moe_swiglu(h, router, w1, w3, w2):
    shp = h.shape
    t = h.reshape(-1, shp[-1])
    logits = (t @ router).astype(jnp.float32)
    top_v, top_i = lax.top_k(logits, TOP_K)
    w = jax.nn.softmax(top_v, axis=-1)
    gates = jnp.einsum('nk,nke->ne', w, jax.nn.one_hot(top_i, N_EXPERTS, dtype=jnp.float32)).astype(h.dtype)
    out = jnp.zeros_like(t)
    for e in range(N_EXPERTS):
        out = out + gates[:, e:e + 1] * swiglu(t, w1[e], w3[e], w2[e])
    return out.reshape(shp)


def setup_inputs(seed: int = 0) -> dict:
    key = jax.random.key(seed)
    ks = jax.random.split(key, 32)
    f32 = jnp.float32
    L = DEPTH

    def nrm(k, shape, scale):
        return jax.random.normal(k, shape, f32) * scale

    a_init = jax.random.uniform(ks[16], (L, 2, LRU_WIDTH), f32, A_MIN, A_MAX)
    a_root = a_init ** (1.0 / LRU_C)
    lru_lambda = jnp.log(a_root) - jnp.log1p(-a_root)
    return {
        'x': nrm(ks[0], (BATCH, SEQ, D_MODEL), 1.0),
        'c': nrm(ks[1], (BATCH, D_MODEL), 1.0),
        'ctx': nrm(ks[2], (BATCH, CTX_LEN, D_MODEL), 1.0),
        'c_ctx': nrm(ks[3], (D_MODEL,), 1.0),
        'w_mod': nrm(ks[4], (L, D_MODEL, N_MOD * D_MODEL), 0.5 * D_MODEL ** -0.5),
        'b_mod': nrm(ks[5], (L, N_MOD * D_MODEL), 0.02),
        'norm1': 1.0 + nrm(ks[6], (L, D_MODEL), 0.02),
        'norm2': 1.0 + nrm(ks[7], (L, D_MODEL), 0.02),
        'w_in': nrm(ks[8], (L, D_MODEL, IN_WIDTH), D_MODEL ** -0.5),
        'w_out': nrm(ks[9], (L, MIX_WIDTH, D_MODEL), MIX_WIDTH ** -0.5),
        'conv_w': nrm(ks[10], (L, CONV_W, LRU_WIDTH), CONV_W ** -0.5),
        'conv_b': nrm(ks[11], (L, LRU_WIDTH), 0.02),
        'lru_wa': nrm(ks[12], (L, 2, LRU_BLOCKS, LRU_BLOCK, LRU_BLOCK), LRU_BLOCK ** -0.5),
        'lru_ba': nrm(ks[13], (L, 2, LRU_WIDTH), 0.02),
        'lru_wi': nrm(ks[14], (L, 2, LRU_BLOCKS, LRU_BLOCK, LRU_BLOCK), LRU_BLOCK ** -0.5),
        'lru_bi': nrm(ks[15], (L, 2, LRU_WIDTH), 0.02),
        'lru_lambda': lru_lambda,
        'hgrn_lb_logits': nrm(ks[17], (2, L, HG_WIDTH), 0.1),
        'hgrn_gnorm': 1.0 + nrm(ks[18], (L, HG_DV), 0.02),
        'q_norm': 1.0 + nrm(ks[19], (L, HEAD_DIM), 0.02),
        'k_norm': 1.0 + nrm(ks[20], (L, HEAD_DIM), 0.02),
        'ffn_w1': nrm(ks[21], (N_DENSE, D_MODEL, D_FF), D_MODEL ** -0.5),
        'ffn_w3': nrm(ks[22], (N_DENSE, D_MODEL, D_FF), D_MODEL ** -0.5),
        'ffn_w2': nrm(ks[23], (N_DENSE, D_FF, D_MODEL), D_FF ** -0.5),
        'router': nrm(ks[24], (N_MOE, D_MODEL, N_EXPERTS), D_MODEL ** -0.5),
        'moe_w1': nrm(ks[25], (N_MOE, N_EXPERTS, D_MODEL, D_FF), D_MODEL ** -0.5),
        'moe_w3': nrm(ks[26], (N_MOE, N_EXPERTS, D_MODEL, D_FF), D_MODEL ** -0.5),
        'moe_w2': nrm(ks[27], (N_MOE, N_EXPERTS, D_FF, D_MODEL), D_FF ** -0.5),
    }


def reference(x, c, ctx, c_ctx, w_mod, b_mod, norm1, norm2, w_in, w_out,
              conv_w, conv_b, lru_wa, lru_ba, lru_wi, lru_bi, lru_lambda,
              hgrn_lb_logits, hgrn_gnorm, q_norm, k_norm,
              ffn_w1, ffn_w3, ffn_w2, router, moe_w1, moe_w3, moe_w2):
    bsz, n, d = x.shape
    n_ctx = ctx.shape[1]
    ROWS = n // GRID_W
    cos, sin = rope_tables(ROWS)
    p = jax.nn.softmax(hgrn_lb_logits.astype(jnp.float32), axis=1)
    lower = jnp.cumsum(p, axis=1) - p[:, :1]
    xl, xc = x, ctx
    for l in range(DEPTH):
        need_ctx = l < DEPTH - 1
        mod_l = (jax.nn.silu(c) @ w_mod[l] + b_mod[l]).reshape(bsz, 1, N_MOD, d)
        mod_c = (jax.nn.silu(c_ctx) @ w_mod[l] + b_mod[l]).reshape(1, 1, N_MOD, d)
        u_l = modulate(rmsnorm(xl, norm1[l]), mod_l, 0) @ w_in[l]
        u_c = modulate(rmsnorm(xc, norm1[l]), mod_c, 0) @ w_in[l]
        ua_l, ub_l, uc_l = jnp.split(u_l, [A_IN, A_IN + B_IN], axis=-1)
        ua_c, ub_c, uc_c = jnp.split(u_c, [A_IN, A_IN + B_IN], axis=-1)
        ra_c, ra_l = mixer_rglru(ua_c, ua_l, conv_w[l], conv_b[l], lru_wa[l], lru_ba[l],
                                 lru_wi[l], lru_bi[l], lru_lambda[l], need_ctx)
        hg_c, hg_l = mixer_hgrn(ub_c, ub_l, lower[0, l], lower[1, l], hgrn_gnorm[l], need_ctx)
        at_c, at_l = mixer_attn(uc_c, uc_l, q_norm[l], k_norm[l], cos, sin, need_ctx)
        xl = xl + mod_l[:, :, 2] * (jnp.concatenate([ra_l, hg_l, at_l], axis=-1) @ w_out[l])
        if need_ctx:
            xc = xc + mod_c[:, :, 2] * (jnp.concatenate([ra_c, hg_c, at_c], axis=-1) @ w_out[l])
            h2 = jnp.concatenate([modulate(rmsnorm(xc, norm2[l]), mod_c, 3),
                                  modulate(rmsnorm(xl, norm2[l]), mod_l, 3)], axis=1)
        else:
            h2 = modulate(rmsnorm(xl, norm2[l]), mod_l, 3)
        j = l // 2
        if l % 2 == 0:
            f = swiglu(h2, ffn_w1[j], ffn_w3[j], ffn_w2[j])
        else:
            f = moe_swiglu(h2, router[j], moe_w1[j], moe_w3[j], moe_w2[j])
        if need_ctx:
            xc = xc + mod_c[:, :, 5] * f[:, :n_ctx]
            xl = xl + mod_l[:, :, 5] * f[:, n_ctx:]
        else:
            xl = xl + mod_l[:, :, 5] * f
    return xl
```

```python
import numpy as np
from contextlib import ExitStack
import concourse.bass as bass
import concourse.mybir as mybir
from concourse.bass_utils import run_bass_kernel_spmd

F32 = mybir.dt.float32
BF16 = mybir.dt.bfloat16
AF = mybir.ActivationFunctionType
ALU = mybir.AluOpType
AX = mybir.AxisListType

D = 2048
NCTX = 256
NLAT = 4096
T = NCTX + NLAT
NT = T // 128
KC = D // 128
DEPTH = 4
DFF = 5632
FC = DFF // 128
NE = 8
EPS = 1e-6
IN_W = 5120
ATT_SCALE = 128 ** -0.5

BLOCKS = [(0, 256)] + [(256 + 512 * i, 512) for i in range(8)]


class Tok:
    __slots__ = ("w", "r")

    def __init__(self):
        self.w = {}
        self.r = {}


class Op:
    __slots__ = ("eng", "fn", "deps", "dma", "sem", "val", "need", "done")

    def __init__(self, eng, fn, dma):
        self.eng = eng
        self.fn = fn
        self.dma = dma
        self.deps = []
        self.sem = None
        self.val = 0
        self.need = False
        self.done = False


ENGS = ("pe", "act", "dve", "pool", "sp")


class Kern:
    def __init__(self, nc, stack):
        self.nc = nc
        self.stack = stack
        self.esem = {e: stack.enter_context(nc.semaphore("s_" + e)) for e in ("pe", "act", "dve", "pool")}
        self.ecnt = {e: 0 for e in self.esem}
        self.dpool = {
            "sp": [stack.enter_context(nc.semaphore("d_sp%d" % i)) for i in range(32)],
            "pool": [stack.enter_context(nc.semaphore("d_pl%d" % i)) for i in range(16)],
            "act": [stack.enter_context(nc.semaphore("d_ac%d" % i)) for i in range(12)],
        }
        self.dnext = {q: 0 for q in self.dpool}
        self.duse = {}
        self.dlast = {}
        self.known = {e: {} for e in ENGS}
        self.nins = 0

    def sb(self, stack, name, shape, dt):
        return stack.enter_context(self.nc.sbuf_tensor(name, list(shape), dt))

    def ps(self, stack, name, shape, dt=F32):
        return stack.enter_context(self.nc.psum_tensor(name, list(shape), dt))


class Phase:
    def __init__(self, K, name):
        self.K = K
        self.nc = K.nc
        self.name = name
        self.stack = ExitStack()
        self.ops = {e: [] for e in ENGS}
        self.uid = 0

    def sb(self, name, shape, dt):
        self.uid += 1
        return self.K.sb(self.stack, "%s_%s%d" % (self.name, name, self.uid), shape, dt)

    def ps(self, name, shape, dt=F32):
        self.uid += 1
        return self.K.ps(self.stack, "%s_%s%d" % (self.name, name, self.uid), shape, dt)

    def rec(self, eng, fn, reads=(), writes=(), dma=False):
        op = Op(eng, fn, dma)
        key = ("d", id(op)) if dma else eng
        deps = {}
        for t in reads:
            for w in t.w.values():
                deps[id(w)] = w
        for t in writes:
            if t.r:
                for r in t.r.values():
                    deps[id(r)] = r
            for k, w in t.w.items():
                if k == key:
                    continue
                if dma and w.dma:
                    continue
                deps[id(w)] = w
        for t in reads:
            t.r[key] = op
        for t in writes:
            if t.r and not (len(t.r) == 1 and key in t.r and False):
                rr = t.r
                t.w = {}
                t.r = {}
                if key in rr and rr[key] is op:
                    pass
            t.w[key] = op
        if dma:
            K = self.K
            pool = K.dpool[eng]
            s = pool[K.dnext[eng] % len(pool)]
            K.dnext[eng] += 1
            prev = K.dlast.get(id(s))
            if prev is not None:
                deps[id(prev)] = prev
            K.duse[id(s)] = K.duse.get(id(s), 0) + 1
            op.sem = s
            op.val = 16 * K.duse[id(s)]
            K.dlast[id(s)] = op
        op.deps = [d for d in deps.values() if not d.done and d is not op]
        for d in op.deps:
            d.need = True
        self.ops[eng].append(op)
        return op

    def pe(self, fn, reads=(), writes=()):
        return self.rec("pe", fn, reads, writes)

    def act(self, fn, reads=(), writes=()):
        return self.rec("act", fn, reads, writes)

    def dve(self, fn, reads=(), writes=()):
        return self.rec("dve", fn, reads, writes)

    def pool(self, fn, reads=(), writes=()):
        return self.rec("pool", fn, reads, writes)

    def load(self, out, in_, writes=(), reads=(), q="sp", **kw):
        return self.rec(q, lambda e: e.dma_start(out=out, in_=in_, **kw), reads, writes, dma=True)

    def store(self, out, in_, reads=(), writes=(), q="pool", **kw):
        return self.rec(q, lambda e: e.dma_start(out=out, in_=in_, **kw), reads, writes, dma=True)

    def finish(self):
        K = self.K
        nc = self.nc
        for e in ("pe", "act", "dve", "pool"):
            for op in self.ops[e]:
                if not op.dma and op.need:
                    K.ecnt[e] += 1
                    op.sem = K.esem[e]
                    op.val = K.ecnt[e]

        def emit(ename, eng):
            known = K.known[ename]
            mydma = {}
            for op in self.ops[ename]:
                for d in op.deps:
                    if d.done:
                        continue
                    sid = id(d.sem)
                    if known.get(sid, 0) < d.val:
                        eng.wait_ge(d.sem, d.val)
                        known[sid] = d.val
                        K.nins += 1
                ins = op.fn(eng)
                K.nins += 1
                if op.dma:
                    ins.then_inc(op.sem, 16)
                    mydma[id(op.sem)] = (op.sem, op.val)
                elif op.need:
                    ins.then_inc(op.sem, 1)
            for sid, (s, v) in mydma.items():
                if known.get(sid, 0) < v:
                    eng.wait_ge(s, v)
                    known[sid] = v

        with nc.Block() as block:
            @block.tensor
            def _(e):
                emit("pe", e)

            @block.scalar
            def _(e):
                emit("act", e)

            @block.vector
            def _(e):
                emit("dve", e)

            @block.gpsimd
            def _(e):
                emit("pool", e)

            @block.sync
            def _(e):
                emit("sp", e)

        for e in ENGS:
            for op in self.ops[e]:
                op.done = True
        allk = {}
        for e in ("pe", "act", "dve", "pool"):
            allk[id(K.esem[e])] = K.ecnt[e]
        for q, pool in K.dpool.items():
            for s in pool:
                allk[id(s)] = 16 * K.duse.get(id(s), 0)
        for e in ENGS:
            K.known[e] = dict(allk)
        self.stack.close()


def bc_last(ap, n):
    return bass.AP(ap.tensor, ap.offset, [list(x) for x in ap.ap] + [[0, n]])


def _colvec(v):
    v = np.asarray(v, np.float32)
    return np.ascontiguousarray(v.reshape(-1, 128).T)


class PLayout:
    def __init__(self):
        self.off = {}
        self.n = 0

    def add(self, name, w):
        self.off[name] = (self.n, w)
        self.n += w


def param_layout():
    P = PLayout()
    for l in range(DEPTH):
        P.add("norm1_%d" % l, 16)
        P.add("norm2_%d" % l, 16)
        P.add("convw_%d" % l, 16)
        P.add("convb_%d" % l, 4)
        P.add("lruba_%d" % l, 8)
        P.add("lrubi_%d" % l, 8)
        P.add("lrulam_%d" % l, 8)
        P.add("gnorm_%d" % l, 1)
    P.add("lblog", 2 * DEPTH * 4)
    P.add("cT", 16)
    P.add("cctxT", 16)
    return P


PL = param_layout()


def pack_params(inp, b):
    P = np.zeros((128, PL.n), np.float32)

    def put(name, arr):
        o, w = PL.off[name]
        arr = np.asarray(arr, np.float32).reshape(128, w)
        P[:, o:o + w] = arr

    for l in range(DEPTH):
        put("norm1_%d" % l, _colvec(inp["norm1"][l]))
        put("norm2_%d" % l, _colvec(inp["norm2"][l]))
        cw = np.stack([_colvec(inp["conv_w"][l][k]) for k in range(4)], axis=1)
        put("convw_%d" % l, cw)
        put("convb_%d" % l, _colvec(inp["conv_b"][l]))
        put("lruba_%d" % l, np.stack([_colvec(inp["lru_ba"][l][d]) for d in range(2)], axis=1))
        put("lrubi_%d" % l, np.stack([_colvec(inp["lru_bi"][l][d]) for d in range(2)], axis=1))
        put("lrulam_%d" % l, np.stack([_colvec(inp["lru_lambda"][l][d]) for d in range(2)], axis=1))
        put("gnorm_%d" % l, np.asarray(inp["hgrn_gnorm"][l], np.float32).reshape(128, 1))
    lb = np.asarray(inp["hgrn_lb_logits"], np.float32)
    lbp = lb.reshape(2, DEPTH, 4, 128).transpose(3, 0, 1, 2)
    put("lblog", lbp)
    put("cT", _colvec(inp["c"][b]))
    put("cctxT", _colvec(inp["c_ctx"]))
    return P


def rope_tables_host():
    F = 32
    inv = (10000.0 ** (-np.arange(F, dtype=np.float32) / F)).astype(np.float32)
    pos = np.arange(NLAT)
    row = (pos // 64).astype(np.float32)
    col = (pos % 64).astype(np.float32)
    ar = row[:, None] * inv[None, :]
    ac = col[:, None] * inv[None, :]
    cosF = np.ones((T, 128), np.float32)
    sinS = np.zeros((T, 128), np.float32)
    cr, sr, cc, sc = np.cos(ar), np.sin(ar), np.cos(ac), np.sin(ac)
    cosF[NCTX:, 0:32] = cr
    cosF[NCTX:, 32:64] = cr
    cosF[NCTX:, 64:96] = cc
    cosF[NCTX:, 96:128] = cc
    sinS[NCTX:, 0:32] = -sr
    sinS[NCTX:, 32:64] = sr
    sinS[NCTX:, 64:96] = -sc
    sinS[NCTX:, 96:128] = sc
    return np.concatenate([cosF, sinS], axis=1).astype(np.float32)


def bc_mid(ap, n):
    a = [list(x) for x in ap.ap]
    return bass.AP(ap.tensor, ap.offset, [a[0], [0, n]] + a[1:])


class Prog:
    def __init__(self, n_layers=DEPTH, debug=(), stop_after=None):
        self.n_layers = n_layers
        self.debug = set(debug)
        self.stop_after = stop_after
        self.nc = bass.Bass("TRN2", target_bir_lowering=False)
        self.stack = ExitStack()
        self.K = Kern(self.nc, self.stack)
        self.dr = {}

    def din(self, name, shape, dt=F32):
        self.dr[name] = self.nc.dram_tensor(name, list(shape), dt, kind="ExternalInput").ap()
        return self.dr[name]

    def dscratch(self, name, shape, dt=F32):
        kind = "ExternalOutput" if name in self.debug else "Internal"
        self.dr[name] = self.nc.dram_tensor(name, list(shape), dt, kind=kind).ap()
        return self.dr[name]

    def declare(self):
        L = DEPTH if self.n_layers > 1 else 1
        NEd = NE if self.n_layers > 1 else 1
        self.din("x", [NLAT, D])
        self.din("ctx", [NCTX, D])
        self.din("params", [128, PL.n])
        self.din("rope", [T, 256])
        self.din("w_mod", [L, D, 6 * D])
        self.din("b_mod", [L, 6 * D])
        self.din("w_in", [L, D, IN_W])
        self.din("w_out", [L, D, D])
        self.din("lru_wa", [L, 2, 4, 128, 128])
        self.din("lru_wi", [L, 2, 4, 128, 128])
        self.din("q_norm", [L, 128])
        self.din("k_norm", [L, 128])
        self.din("ffn_w1", [2, D, DFF])
        self.din("ffn_w3", [2, D, DFF])
        self.din("ffn_w2", [2, DFF, D])
        self.din("router", [2, D, NE])
        self.din("moe_w1", [2, NEd, D, DFF])
        self.din("moe_w3", [2, NEd, D, DFF])
        self.din("moe_w2", [2, NEd, DFF, D])
        self.dr["out"] = self.nc.dram_tensor("out", [NLAT, D], F32, kind="ExternalOutput").ap()
        self.dscratch("X", [T, D])
        self.dscratch("MODROW", [DEPTH, 2, 6 * D])
        self.dscratch("UAT", [1024, T])
        self.dscratch("UBT", [2048, T])
        self.dscratch("UBI", [T, 512], BF16)
        self.dscratch("UC", [T, 1536])
        self.dscratch("MIXT", [2048, T], BF16)
        self.dscratch("QT", [8, 128, T], BF16)
        self.dscratch("KT", [2, 128, T], BF16)
        self.dscratch("V", [T, 256], BF16)
        self.dscratch("H2T", [128, KC, T], BF16)
        self.dscratch("GT", [NE, T])
        if "DBG" in self.debug:
            self.dscratch("DBG", [12, 128, T])
        for e_ in range(NE):
            self.dscratch("AT%d" % e_, [len(BLOCKS), 128, FC, 512], BF16)

    def persistent(self):
        K, st = self.K, self.stack
        self.par = K.sb(st, "par", [128, PL.n], F32)
        self.identF = K.sb(st, "identF", [128, 128], F32)
        self.identB = K.sb(st, "identB", [128, 128], BF16)
        self.onesF = K.sb(st, "onesF", [128, 128], F32)
        self.onesB = K.sb(st, "onesB", [128, 128], BF16)
        self.tri = K.sb(st, "tri", [64, 2, 64], F32)
        self.sel = K.sb(st, "sel", [8, NE, 128], F32)
        self.rmask = K.sb(st, "rmask", [128, 512], F32)
        self.modT = K.sb(st, "modT", [128, DEPTH, 96, 2], F32)
        self.der = K.sb(st, "der", [128, DEPTH, 2, 4, 16], F32)
        self.sp8 = K.sb(st, "sp8", [128, DEPTH, 8], F32)
        self.sp16 = K.sb(st, "sp16", [128, DEPTH, 8], F32)
        self.low = K.sb(st, "low", [128, 2, DEPTH, 4], F32)
        self.oml = K.sb(st, "oml", [128, 2, DEPTH, 4], F32)
        self.sc = K.sb(st, "sc", [128, 16, 2], F32)
        self.epsb = K.sb(st, "epsb", [128, 1], F32)

    def pv(self, name, l=None):
        key = name if l is None else "%s_%d" % (name, l)
        o, w = PL.off[key]
        return o, w

    def prologue(self):
        K, nc, dr = self.K, self.nc, self.dr
        ph = Phase(K, "pro")
        tk = Tok()
        par = self.par
        ph.load(par[:, :], dr["params"][:, :], writes=[tk])
        ph.load(dr["X"][0:NCTX, :], dr["ctx"][:, :])
        for i in range(4):
            ph.load(dr["X"][NCTX + 1024 * i:NCTX + 1024 * (i + 1), :], dr["x"][1024 * i:1024 * (i + 1), :])
        tc_ = Tok()
        onesF, onesB, identF, identB = self.onesF, self.onesB, self.identF, self.identB
        tmp1 = ph.sb("tmp1", [128, 128], F32)
        ph.pool(lambda e: e.memset(tmp1[:, :], 1.0), writes=[tc_])
        ph.pool(lambda e: e.memset(onesF[:, :], 1.0 / 128.0), writes=[tc_])
        ph.pool(lambda e: e.memset(onesB[:, :], 1.0), writes=[tc_])
        tid = Tok()
        ph.pool(lambda e: e.affine_select(identF[:, :], tmp1[:, :], [[-1, 128]], ALU.is_equal, 0.0,
                                          base=0, channel_multiplier=1), reads=[tc_], writes=[tid])
        ph.pool(lambda e: e.tensor_copy(identB[:, :], identF[:, :]), reads=[tid], writes=[tc_])
        tri = self.tri
        ph.pool(lambda e: e.affine_select(tri[:, 0, :], tmp1[0:64, 0:64], [[1, 64]], ALU.is_ge, 0.0,
                                          base=0, channel_multiplier=-1), reads=[tc_], writes=[tid])
        ph.pool(lambda e: e.affine_select(tri[:, 1, :], tmp1[0:64, 0:64], [[-1, 64]], ALU.is_ge, 0.0,
                                          base=0, channel_multiplier=1), reads=[tc_], writes=[tid])
        sel = self.sel
        tmp8 = ph.sb("tmp8", [8, NE, 128], F32)
        ph.pool(lambda e: e.memset(tmp8[:, :, :], 1.0), writes=[tc_])
        ph.pool(lambda e: e.affine_select(sel[:, :, :], tmp8[:, :, :], [[-1, NE], [0, 128]], ALU.is_equal, 0.0,
                                          base=0, channel_multiplier=1), reads=[tc_], writes=[tid])
        rmask = self.rmask
        epsb = self.epsb
        ph.pool(lambda e: e.memset(epsb[:, :], EPS), writes=[tc_])
        ph.pool(lambda e: e.memset(rmask[:, :], 1.0), writes=[tc_])
        ph.pool(lambda e: e.memset(rmask[:, 0:512:16], 0.0), reads=[tc_], writes=[tid])

        sc = self.sc
        oc, _ = self.pv("cT")
        occ, _ = self.pv("cctxT")
        tsc = Tok()
        ph.act(lambda e: e.activation(out=sc[:, :, 0], in_=par[:, oc:oc + 16], func=AF.Silu), reads=[tk], writes=[tsc])
        ph.act(lambda e: e.activation(out=sc[:, :, 1], in_=par[:, occ:occ + 16], func=AF.Silu), reads=[tk], writes=[tsc])

        modT = self.modT
        tmod = Tok()
        NB = 2
        wm = [ph.sb("wm", [128, KC, 512], F32) for _ in range(NB)]
        twm = [Tok() for _ in range(NB)]
        bm = [ph.sb("bm", [2, 512], F32) for _ in range(NB)]
        tbm = [Tok() for _ in range(NB)]
        row = [ph.sb("row", [2, 512], F32) for _ in range(NB)]
        trow = [Tok() for _ in range(NB)]
        psr = [ph.ps("psr", [2, 512]) for _ in range(NB)]
        tpsr = [Tok() for _ in range(NB)]
        pst = [ph.ps("pst", [128, 4, 2]) for _ in range(NB)]
        tpst = [Tok() for _ in range(NB)]
        it = 0
        for l in range(self.n_layers):
            wsrc = dr["w_mod"][l].rearrange("(k p) n -> p k n", p=128)
            for j in range(24):
                b = it % NB
                it += 1
                ph.load(wm[b][:, :, :], wsrc[:, :, j * 512:(j + 1) * 512], writes=[twm[b]])
                ph.load(bm[b][:, :], dr["b_mod"][l, j * 512:(j + 1) * 512].partition_broadcast(2), writes=[tbm[b]])
                for k in range(KC):
                    ph.pe(lambda e, b=b, k=k: e.matmul(psr[b][:, :], sc[:, k, :], wm[b][:, k, :],
                                                       start=(k == 0), stop=(k == KC - 1)),
                          reads=[tsc, twm[b]], writes=[tpsr[b]])
                ph.dve(lambda e, b=b: e.tensor_tensor(row[b][:, :], psr[b][:, :], bm[b][:, :], ALU.add),
                       reads=[tpsr[b], tbm[b]], writes=[trow[b]])
                ph.store(dr["MODROW"][l, :, j * 512:(j + 1) * 512], row[b][:, :], reads=[trow[b]])
                for i in range(4):
                    ph.pe(lambda e, b=b, i=i: e.transpose(pst[b][:, i, :], row[b][:, i * 128:(i + 1) * 128],
                                                          identF[0:2, 0:2]),
                          reads=[trow[b], tid], writes=[tpst[b]])
                ph.act(lambda e, b=b, l=l, j=j: e.copy(modT[:, l, j * 4:(j + 1) * 4, :], pst[b][:, :, :]),
                       reads=[tpst[b]], writes=[tmod])
        der = self.der
        tder = Tok()
        for l in range(self.n_layers):
            o1, _ = self.pv("norm1", l)
            o2, _ = self.pv("norm2", l)
            for ty in range(2):
                ph.dve(lambda e, l=l, ty=ty: e.tensor_scalar(der[:, l, ty, 0, :], modT[:, l, 16:32, ty], 1.0, None,
                                                              ALU.add), reads=[tmod], writes=[tder])
                ph.dve(lambda e, l=l, ty=ty, o1=o1: e.tensor_tensor(der[:, l, ty, 0, :], der[:, l, ty, 0, :],
                                                                     par[:, o1:o1 + 16], ALU.mult),
                       reads=[tder, tk], writes=[tder])
                ph.dve(lambda e, l=l, ty=ty: e.tensor_copy(der[:, l, ty, 1, :], modT[:, l, 0:16, ty]),
                       reads=[tmod], writes=[tder])
                ph.dve(lambda e, l=l, ty=ty: e.tensor_scalar(der[:, l, ty, 2, :], modT[:, l, 64:80, ty], 1.0, None,
                                                              ALU.add), reads=[tmod], writes=[tder])
                ph.dve(lambda e, l=l, ty=ty, o2=o2: e.tensor_tensor(der[:, l, ty, 2, :], der[:, l, ty, 2, :],
                                                                     par[:, o2:o2 + 16], ALU.mult),
                       reads=[tder, tk], writes=[tder])
                ph.dve(lambda e, l=l, ty=ty: e.tensor_copy(der[:, l, ty, 3, :], modT[:, l, 48:64, ty]),
                       reads=[tmod], writes=[tder])
        sp8, sp16 = self.sp8, self.sp16
        tsp = Tok()
        for l in range(self.n_layers):
            ol, _ = self.pv("lrulam", l)
            ph.act(lambda e, l=l, ol=ol: e.activation(out=sp8[:, l, :], in_=par[:, ol:ol + 8], func=AF.Exp, scale=-1.0),
                   reads=[tk], writes=[tsp])
            ph.act(lambda e, l=l: e.activation(out=sp8[:, l, :], in_=sp8[:, l, :], func=AF.Ln, bias=1.0),
                   reads=[tsp], writes=[tsp])
            ph.dve(lambda e, l=l: e.tensor_scalar(sp16[:, l, :], sp8[:, l, :], -16.0, None, ALU.mult),
                   reads=[tsp], writes=[tsp])
            ph.dve(lambda e, l=l: e.tensor_scalar(sp8[:, l, :], sp8[:, l, :], -8.0, None, ALU.mult),
                   reads=[tsp], writes=[tsp])
        low, oml = self.low, self.oml
        olb, _ = self.pv("lblog")
        ex = ph.sb("ex", [128, 2, DEPTH, 4], F32)
        mx = ph.sb("mx", [128, 2, 4], F32)
        sm = ph.sb("sm", [128, 2, 4], F32)
        tl = Tok()
        lbv = par[:, olb:olb + 2 * DEPTH * 4].rearrange("p (d l h) -> p d l h", d=2, l=DEPTH)
        ph.dve(lambda e: e.tensor_tensor(mx[:, :, :], lbv[:, :, 0, :], lbv[:, :, 1, :], ALU.max), reads=[tk], writes=[tl])
        for l in range(2, DEPTH):
            ph.dve(lambda e, l=l: e.tensor_tensor(mx[:, :, :], mx[:, :, :], lbv[:, :, l, :], ALU.max), reads=[tl, tk], writes=[tl])
        for l in range(DEPTH):
            ph.dve(lambda e, l=l: e.tensor_tensor(ex[:, :, l, :], lbv[:, :, l, :], mx[:, :, :], ALU.subtract),
                   reads=[tl, tk], writes=[tl])
        ph.act(lambda e: e.activation(out=ex[:, :, :, :], in_=ex[:, :, :, :], func=AF.Exp), reads=[tl], writes=[tl])
        ph.dve(lambda e: e.tensor_tensor(sm[:, :, :], ex[:, :, 0, :], ex[:, :, 1, :], ALU.add), reads=[tl], writes=[tl])
        for l in range(2, DEPTH):
            ph.dve(lambda e, l=l: e.tensor_tensor(sm[:, :, :], sm[:, :, :], ex[:, :, l, :], ALU.add), reads=[tl], writes=[tl])
        ph.dve(lambda e: e.reciprocal(sm[:, :, :], sm[:, :, :]), reads=[tl], writes=[tl])
        ph.dve(lambda e: e.memset(low[:, :, 0, :], 0.0), writes=[tl])
        for l in range(1, DEPTH):
            ph.dve(lambda e, l=l: e.tensor_tensor(ex[:, :, l, :], ex[:, :, l, :], sm[:, :, :], ALU.mult), reads=[tl], writes=[tl])
            ph.dve(lambda e, l=l: e.tensor_tensor(low[:, :, l, :], low[:, :, l - 1, :], ex[:, :, l, :], ALU.add),
                   reads=[tl], writes=[tl])
        ph.dve(lambda e: e.tensor_scalar(oml[:, :, :, :], low[:, :, :, :], -1.0, 1.0, ALU.mult, ALU.add), reads=[tl], writes=[tl])
        ph.finish()


SHARED = ["w_mod", "b_mod", "w_in", "w_out", "lru_wa", "lru_wi", "q_norm", "k_norm", "ffn_w1", "ffn_w3", "ffn_w2",
          "router", "moe_w1", "moe_w3", "moe_w2"]


def make_in_maps(inp, cores, small=False):
    rope = rope_tables_host()
    if small:
        shared = {}
        for k in SHARED:
            a = inp[k]
            if k.startswith("moe_"):
                a = a[:, 0:1]
            elif k in ("w_mod", "b_mod", "w_in", "w_out", "lru_wa", "lru_wi", "q_norm", "k_norm"):
                a = a[0:1]
            shared[k] = np.ascontiguousarray(np.asarray(a, np.float32))
    else:
        shared = {k: np.ascontiguousarray(np.asarray(inp[k], np.float32)) for k in SHARED}
    maps = []
    for b in cores:
        m = dict(shared)
        m["x"] = np.ascontiguousarray(np.asarray(inp["x"][b], np.float32))
        m["ctx"] = np.ascontiguousarray(np.asarray(inp["ctx"][b], np.float32))
        m["params"] = pack_params(inp, b)
        m["rope"] = rope
        maps.append(m)
    return maps


def build_program(n_layers=DEPTH, debug=(), stop_after=None):
    P = Prog(n_layers, debug, stop_after)
    P.declare()
    P.persistent()
    P.build()
    return P


def kernel(**inputs):
    P = build_program()
    maps = make_in_maps(inputs, list(range(8)))
    res = run_bass_kernel_spmd(P.nc, maps, core_ids=list(range(8)))
    out = np.stack([np.asarray(r["out"], np.float32) for r in res.results], axis=0)
    return out


GROUPS = [BLOCKS[0:5], BLOCKS[5:9]]
TGMAX = 2304


def _build(self):
    self.prologue()
    if self.stop_after == "pro":
        return self.epilogue(False)
    for l in range(self.n_layers):
        for name, fn in (("p1", self.phase_p1), ("p2", self.phase_p2), ("p3", self.phase_p3),
                         ("p4a", self.phase_p4a), ("p4b", self.phase_p4b), ("p5", self.phase_p5),
                         ("p6a", self.phase_p6a), ("p6b", self.phase_p6b)):
            fn(l)
            if self.stop_after == "%s_%d" % (name, l):
                return self.epilogue(False)
    return self.epilogue(True)


def _epilogue(self, full):
    K, dr = self.K, self.dr
    ph = Phase(K, "epi")
    for i in range(4):
        ph.load(dr["out"][1024 * i:1024 * (i + 1), :], dr["X"][NCTX + 1024 * i:NCTX + 1024 * (i + 1), :])
    ph.finish()
    self.stack.close()


def norm_tiles(self, ph, l, which, grp_blocks, hT, thT, f32_out=None):
    K, dr = self.K, self.dr
    der, identF = self.der, self.identF
    st = ph._norm_state if hasattr(ph, "_norm_state") else None
    if st is None:
        st = {}
        st["xt"] = [ph.sb("xt", [128, D], F32) for _ in range(2)]
        st["txt"] = [Tok() for _ in range(2)]
        st["xn"] = [ph.sb("xn", [128, D], F32) for _ in range(2)]
        st["txn"] = [Tok() for _ in range(2)]
        st["junk"] = ph.sb("junk", [128, D], F32)
        st["tjunk"] = Tok()
        st["ss"] = [ph.sb("ss", [128, 1], F32) for _ in range(2)]
        st["rs"] = [ph.sb("rs", [128, 1], F32) for _ in range(2)]
        st["tss"] = [Tok() for _ in range(2)]
        st["trs"] = [Tok() for _ in range(2)]
        st["pT"] = [ph.ps("pT", [128, 4, 128]) for _ in range(4)]
        st["tpT"] = [Tok() for _ in range(4)]
        st["i"] = 0
        ph._norm_state = st
    g0 = grp_blocks[0][0]
    for (b0, w) in grp_blocks:
        for tt in range(w // 128):
            t0 = b0 + tt * 128
            ty = 1 if t0 < NCTX else 0
            col = t0 - g0
            i = st["i"]
            st["i"] += 1
            p = i % 2
            xt, txt, xn, txn = st["xt"][p], st["txt"][p], st["xn"][p], st["txn"][p]
            ss, rs, tss, trs = st["ss"][p], st["rs"][p], st["tss"][p], st["trs"][p]
            junk, tjunk = st["junk"], st["tjunk"]
            ph.load(xt[:, :], dr["X"][t0:t0 + 128, :], writes=[txt])
            ph.act(lambda e, xt=xt, ss=ss: e.activation(out=junk[:, :], in_=xt[:, :], func=AF.Square, accum_out=ss[:, :]),
                   reads=[txt], writes=[tjunk, tss])
            ph.act(lambda e, ss=ss, rs=rs: e.activation(out=rs[:, :], in_=ss[:, :], func=AF.Sqrt, scale=1.0 / D, bias=self.epsb[:, :]),
                   reads=[tss], writes=[trs])
            ph.dve(lambda e, rs=rs: e.reciprocal(rs[:, :], rs[:, :]), reads=[trs], writes=[trs])
            ph.dve(lambda e, xn=xn, xt=xt, rs=rs: e.tensor_scalar(xn[:, :], xt[:, :], rs[:, :], None, ALU.mult),
                   reads=[txt, trs], writes=[txn])
            for k in range(KC):
                ph.pe(lambda e, k=k, xn=xn: e.transpose(st["pT"][k // 4][:, k % 4, :], xn[:, k * 128:(k + 1) * 128], identF[:, :]),
                      reads=[txn], writes=[st["tpT"][k // 4]])
            for k in range(KC):
                gsv = der[:, l, ty, 2 * which, k:k + 1]
                shv = der[:, l, ty, 2 * which + 1, k:k + 1]
                src = st["pT"][k // 4][:, k % 4, :]
                dst = hT[:, k, col:col + 128]
                if k % 2 == 0:
                    ph.dve(lambda e, dst=dst, src=src, gsv=gsv, shv=shv: e.tensor_scalar(dst, src, gsv, shv, ALU.mult, ALU.add),
                           reads=[st["tpT"][k // 4]], writes=[thT])
                else:
                    ph.act(lambda e, dst=dst, src=src, gsv=gsv, shv=shv: e.activation(out=dst, in_=src, func=AF.Identity, scale=gsv, bias=shv),
                           reads=[st["tpT"][k // 4]], writes=[thT])
            if f32_out is not None:
                f32_out(t0, col, ty, st)


def phase_p1(self, l):
    K, dr = self.K, self.dr
    ph = Phase(K, "p1_%d" % l)
    hT = ph.sb("hT", [128, KC, TGMAX], BF16)
    thT = Tok()
    CW = 256
    wst = [ph.sb("wst", [128, KC, CW], F32) for _ in range(2)]
    twst = [Tok() for _ in range(2)]
    wb = [ph.sb("wb", [128, KC, CW], BF16) for _ in range(2)]
    twb = [Tok() for _ in range(2)]
    pm = [ph.ps("pm", [128, 512]) for _ in range(4)]
    tpm = [Tok() for _ in range(4)]
    ost = [ph.sb("ost", [128, 512], F32) for _ in range(4)]
    tost = [Tok() for _ in range(4)]
    ostb = [ph.sb("ostb", [128, 256], BF16) for _ in range(2)]
    tostb = [Tok() for _ in range(2)]
    wsrc = dr["w_in"][l].rearrange("(k p) n -> p k n", p=128)
    cnt = {"w": 0, "m": 0, "b": 0}
    for grp in GROUPS:
        g0 = grp[0][0]
        norm_tiles(self, ph, l, 0, grp, hT, thT)
        for cg in range(IN_W // CW):
            c0 = cg * CW
            j = cnt["w"] % 2
            cnt["w"] += 1
            ph.load(wst[j][:, :, :], wsrc[:, :, c0:c0 + CW], writes=[twst[j]])
            ph.pool(lambda e, j=j: e.tensor_copy(wb[j][:, :, :], wst[j][:, :, :]), reads=[twst[j]], writes=[twb[j]])
            tokmajor = (2560 <= c0 < 3072) or (c0 >= 3584)
            if not tokmajor:
                if c0 < 1024:
                    dst, r0 = dr["UAT"], c0
                elif c0 < 2560:
                    dst, r0 = dr["UBT"], c0 - 1024
                else:
                    dst, r0 = dr["UBT"], c0 - 3072 + 1536
                for (b0, w) in grp:
                    col = b0 - g0
                    for m in range(CW // 128):
                        q = cnt["m"] % 4
                        cnt["m"] += 1
                        for k in range(KC):
                            ph.pe(lambda e, q=q, j=j, k=k, m=m, col=col, w=w: e.matmul(
                                pm[q][:, 0:w], wb[j][:, k, m * 128:(m + 1) * 128], hT[:, k, col:col + w],
                                start=(k == 0), stop=(k == KC - 1)), reads=[twb[j], thT], writes=[tpm[q]])
                        if q % 2 == 0:
                            ph.act(lambda e, q=q, w=w: e.copy(ost[q][:, 0:w], pm[q][:, 0:w]), reads=[tpm[q]], writes=[tost[q]])
                        else:
                            ph.dve(lambda e, q=q, w=w: e.tensor_copy(ost[q][:, 0:w], pm[q][:, 0:w]), reads=[tpm[q]], writes=[tost[q]])
                        ph.store(dst[r0 + m * 128:r0 + (m + 1) * 128, b0:b0 + w], ost[q][:, 0:w], reads=[tost[q]])
            else:
                for (b0, w) in grp:
                    for tt in range(w // 128):
                        t0 = b0 + tt * 128
                        col = t0 - g0
                        q = cnt["m"] % 4
                        cnt["m"] += 1
                        for k in range(KC):
                            ph.pe(lambda e, q=q, j=j, k=k, col=col: e.matmul(
                                pm[q][:, 0:CW], hT[:, k, col:col + 128], wb[j][:, k, :],
                                start=(k == 0), stop=(k == KC - 1)), reads=[twb[j], thT], writes=[tpm[q]])
                        if c0 < 3072:
                            bi = cnt["b"] % 2
                            cnt["b"] += 1
                            ph.act(lambda e, q=q, bi=bi: e.copy(ostb[bi][:, :], pm[q][:, 0:CW]), reads=[tpm[q]], writes=[tostb[bi]])
                            ph.store(dr["UBI"][t0:t0 + 128, c0 - 2560:c0 - 2560 + CW], ostb[bi][:, :], reads=[tostb[bi]])
                        else:
                            if q % 2 == 0:
                                ph.act(lambda e, q=q: e.copy(ost[q][:, 0:CW], pm[q][:, 0:CW]), reads=[tpm[q]], writes=[tost[q]])
                            else:
                                ph.dve(lambda e, q=q: e.tensor_copy(ost[q][:, 0:CW], pm[q][:, 0:CW]), reads=[tpm[q]], writes=[tost[q]])
                            ph.store(dr["UC"][t0:t0 + 128, c0 - 3584:c0 - 3584 + CW], ost[q][:, 0:CW], reads=[tost[q]])
    ph.finish()


def _stub(self, l):
    pass


for _n in ("phase_p2", "phase_p3", "phase_p4a", "phase_p4b", "phase_p5", "phase_p6a", "phase_p6b"):
    setattr(Prog, _n, _stub)
Prog.build = _build
Prog.epilogue = _epilogue
Prog.phase_p1 = phase_p1


def rev(ap):
    return ap[:, ::-1]


def phase_p2(self, l):
    K, dr, par = self.K, self.dr, self.par
    ph = Phase(K, "p2_%d" % l)
    names = ["yb", "xb", "xc", "r", "ii", "a", "m", "hf", "hb"]
    tl = {n: ph.sb(n, [128, T], F32) for n in names}
    tk = {n: Tok() for n in names}
    ob = ph.sb("ob", [128, T], BF16)
    tob = Tok()
    wa = ph.sb("wa", [128, 2, 4, 128], F32)
    wi = ph.sb("wi", [128, 2, 4, 128], F32)
    tw = Tok()
    ph.load(wa[:, :, :, :], dr["lru_wa"][l].rearrange("d n k j -> k d n j"), writes=[tw])
    ph.load(wi[:, :, :, :], dr["lru_wi"][l].rearrange("d n k j -> k d n j"), writes=[tw])
    pg = [ph.ps("pg", [128, 512]) for _ in range(4)]
    tpg = [Tok() for _ in range(4)]
    ocw, _ = self.pv("convw", l)
    ocb, _ = self.pv("convb", l)
    oba, _ = self.pv("lruba", l)
    obi, _ = self.pv("lrubi", l)
    sp8, sp16 = self.sp8, self.sp16
    yb, xb, xc, r, ii, a, m, hf, hb = [tl[n] for n in names]
    cnt = 0
    for n in range(4):
        ph.load(yb[:, :], dr["UAT"][n * 128:(n + 1) * 128, :], writes=[tk["yb"]])
        ph.load(xb[:, :], dr["UAT"][512 + n * 128:512 + (n + 1) * 128, :], writes=[tk["xb"]])
        cw = lambda k, n=n: par[:, ocw + k * 4 + n:ocw + k * 4 + n + 1]
        cb = par[:, ocb + n:ocb + n + 1]
        ph.dve(lambda e, cw=cw, cb=cb: e.tensor_scalar(xc[:, :], xb[:, :], cw(2), cb, ALU.mult, ALU.add),
               reads=[tk["xb"]], writes=[tk["xc"]])
        for k in (0, 1, 3):
            o = k - 2
            for (s0, s1) in ((0, NCTX), (NCTX, T)):
                lo = max(s0, s0 - o)
                hi = min(s1, s1 - o)
                ph.dve(lambda e, cw=cw, k=k, lo=lo, hi=hi, o=o: e.scalar_tensor_tensor(
                    xc[:, lo:hi], xb[:, lo + o:hi + o], cw(k), xc[:, lo:hi], ALU.mult, ALU.add),
                    reads=[tk["xb"], tk["xc"]], writes=[tk["xc"]])
        for d in range(2):
            ba = par[:, oba + d * 4 + n:oba + d * 4 + n + 1]
            bi = par[:, obi + d * 4 + n:obi + d * 4 + n + 1]
            for (b0, w) in BLOCKS:
                q = cnt % 4
                cnt += 1
                ph.pe(lambda e, q=q, d=d, n=n, b0=b0, w=w: e.matmul(pg[q][:, 0:w], wa[:, d, n, :], xc[:, b0:b0 + w], start=True, stop=True),
                      reads=[tw, tk["xc"]], writes=[tpg[q]])
                ph.act(lambda e, q=q, b0=b0, w=w, ba=ba: e.activation(out=r[:, b0:b0 + w], in_=pg[q][:, 0:w], func=AF.Sigmoid, bias=ba),
                       reads=[tpg[q]], writes=[tk["r"]])
                q = cnt % 4
                cnt += 1
                ph.pe(lambda e, q=q, d=d, n=n, b0=b0, w=w: e.matmul(pg[q][:, 0:w], wi[:, d, n, :], xc[:, b0:b0 + w], start=True, stop=True),
                      reads=[tw, tk["xc"]], writes=[tpg[q]])
                ph.act(lambda e, q=q, b0=b0, w=w, bi=bi: e.activation(out=ii[:, b0:b0 + w], in_=pg[q][:, 0:w], func=AF.Sigmoid, bias=bi),
                       reads=[tpg[q]], writes=[tk["ii"]])
            s8 = sp8[:, l, d * 4 + n:d * 4 + n + 1]
            s16 = sp16[:, l, d * 4 + n:d * 4 + n + 1]
            ph.act(lambda e, s8=s8: e.activation(out=a[:, :], in_=r[:, :], func=AF.Exp, scale=s8), reads=[tk["r"]], writes=[tk["a"]])
            ph.act(lambda e, s16=s16: e.activation(out=m[:, :], in_=r[:, :], func=AF.Exp, scale=s16), reads=[tk["r"]], writes=[tk["m"]])
            ph.dve(lambda e: e.tensor_scalar(m[:, :], m[:, :], -1.0, 1.0, ALU.mult, ALU.add), reads=[tk["m"]], writes=[tk["m"]])
            ph.act(lambda e: e.activation(out=m[:, :], in_=m[:, :], func=AF.Sqrt), reads=[tk["m"]], writes=[tk["m"]])
            ph.dve(lambda e: e.tensor_tensor(ii[:, :], ii[:, :], xc[:, :], ALU.mult), reads=[tk["ii"], tk["xc"]], writes=[tk["ii"]])
            ph.dve(lambda e: e.tensor_tensor(ii[:, :], ii[:, :], m[:, :], ALU.mult), reads=[tk["ii"], tk["m"]], writes=[tk["ii"]])
            if d == 0:
                ph.dve(lambda e: e.tensor_tensor_scan(hf[:, :], a[:, :], ii[:, :], 0.0, ALU.mult, ALU.add),
                       reads=[tk["a"], tk["ii"]], writes=[tk["hf"]])
            else:
                ph.dve(lambda e: e.tensor_tensor_scan(rev(hb[:, 0:NCTX]), rev(a[:, 0:NCTX]), rev(ii[:, 0:NCTX]), 0.0, ALU.mult, ALU.add),
                       reads=[tk["a"], tk["ii"]], writes=[tk["hb"]])
                ph.dve(lambda e: e.tensor_tensor_scan(rev(hb[:, NCTX:T]), rev(a[:, NCTX:T]), rev(ii[:, NCTX:T]), hb[:, 0:1], ALU.mult, ALU.add),
                       reads=[tk["a"], tk["ii"], tk["hb"]], writes=[tk["hb"]])
        ph.dve(lambda e: e.tensor_tensor(r[:, :], yb[:, :], yb[:, :], ALU.mult), reads=[tk["yb"], tk["r"]], writes=[tk["r"]])
        ph.dve(lambda e: e.tensor_scalar(r[:, :], r[:, :], 0.044715, 1.0, ALU.mult, ALU.add), reads=[tk["r"]], writes=[tk["r"]])
        ph.dve(lambda e: e.tensor_tensor(r[:, :], r[:, :], yb[:, :], ALU.mult), reads=[tk["r"], tk["yb"]], writes=[tk["r"]])
        ph.act(lambda e: e.activation(out=r[:, :], in_=r[:, :], func=AF.Sigmoid, scale=1.5957691216), reads=[tk["r"]], writes=[tk["r"]])
        ph.dve(lambda e: e.tensor_tensor(r[:, :], r[:, :], yb[:, :], ALU.mult), reads=[tk["r"], tk["yb"]], writes=[tk["r"]])
        ph.dve(lambda e: e.tensor_tensor(hf[:, :], hf[:, :], hb[:, :], ALU.add), reads=[tk["hf"], tk["hb"]], writes=[tk["hf"]])
        ph.dve(lambda e: e.tensor_tensor(ob[:, :], hf[:, :], r[:, :], ALU.mult), reads=[tk["hf"], tk["r"]], writes=[tob])
        ph.store(dr["MIXT"][n * 128:(n + 1) * 128, :], ob[:, :], reads=[tob])
    ph.finish()


Prog.phase_p2 = phase_p2


CH = 16


def phase_p3(self, l):
    K, dr, par = self.K, self.dr, self.par
    ph = Phase(K, "p3_%d" % l)
    identB, onesF, tri, rmask, low, oml = self.identB, self.onesF, self.tri, self.rmask, self.low, self.oml
    ogn, _ = self.pv("gnorm", l)
    of_ = ph.sb("of", [128, T], F32)
    tof = Tok()
    Vh = ph.sb("Vh", [CH, T // CH, 128], BF16)
    tV = Tok()
    S = ph.sb("S", [128, 128], F32)
    Sb = [ph.sb("Sb", [128, 128], BF16) for _ in range(2)]
    tS = Tok()
    tSb = [Tok() for _ in range(2)]
    sp = 0
    nm = ["qr", "fl", "q", "f", "g", "k", "bc", "e1", "e2", "og", "sq", "rs", "t1"]
    bl = {n: ph.sb(n, [128, 512], F32) for n in nm}
    tb = {n: Tok() for n in nm}
    Qt = ph.sb("Qt", [128, 512], BF16)
    Kt = ph.sb("Kt", [128, 512], BF16)
    Kh = ph.sb("Kh", [128, 512], BF16)
    tQt, tKt, tKh = Tok(), Tok(), Tok()
    yb = ph.sb("yb", [128, 512], BF16)
    tyb = Tok()
    ATm = [ph.sb("ATm", [CH, CH], BF16) for _ in range(2)]
    tATm = [Tok() for _ in range(2)]
    KhT = [ph.sb("KhT", [CH, 128], BF16) for _ in range(2)]
    tKhT = [Tok() for _ in range(2)]
    psA = [ph.ps("psA", [CH, CH]) for _ in range(2)]
    tpsA = [Tok() for _ in range(2)]
    psT = [ph.ps("psT", [CH, 128], BF16) for _ in range(2)]
    tpsT = [Tok() for _ in range(2)]
    psKV = [ph.ps("psKV", [128, 128]) for _ in range(2)]
    tpsKV = [Tok() for _ in range(2)]
    psO = ph.ps("psO", [128, 512])
    tpsO = Tok()
    psM = ph.ps("psM", [128, 512])
    tpsM = Tok()
    cc = 0
    for h in range(4):
        ph.load(Vh[:, :, :], dr["UBI"][:, h * 128:(h + 1) * 128].rearrange("(c s) v -> s c v", s=CH), writes=[tV])
        for d in range(2):
            lowv = low[:, d, l, h:h + 1]
            omlv = oml[:, d, l, h:h + 1]
            ph.dve(lambda e: e.memset(S[:, :], 0.0), writes=[tS])
            ph.dve(lambda e, sp=sp: e.memset(Sb[sp][:, :], 0.0), writes=[tSb[sp]])
            order = list(range(9)) if d == 0 else [0] + list(range(8, 0, -1))
            for bi in order:
                b0, w = BLOCKS[bi]
                nch = w // CH
                ph.load(bl["qr"][:, 0:w], dr["UBT"][h * 128:(h + 1) * 128, b0:b0 + w], writes=[tb["qr"]])
                ph.load(bl["fl"][:, 0:w], dr["UBT"][512 + d * 512 + h * 128:512 + d * 512 + (h + 1) * 128, b0:b0 + w], writes=[tb["fl"]])
                ph.act(lambda e, w=w: e.activation(out=bl["q"][:, 0:w], in_=bl["qr"][:, 0:w], func=AF.Silu), reads=[tb["qr"]], writes=[tb["q"]])
                ph.act(lambda e, w=w: e.activation(out=bl["f"][:, 0:w], in_=bl["fl"][:, 0:w], func=AF.Sigmoid), reads=[tb["fl"]], writes=[tb["f"]])
                ph.dve(lambda e, w=w, lowv=lowv, omlv=omlv: e.tensor_scalar(bl["f"][:, 0:w], bl["f"][:, 0:w], omlv, lowv, ALU.mult, ALU.add),
                       reads=[tb["f"]], writes=[tb["f"]])
                ph.act(lambda e, w=w: e.activation(out=bl["g"][:, 0:w], in_=bl["f"][:, 0:w], func=AF.Ln), reads=[tb["f"]], writes=[tb["g"]])
                ph.dve(lambda e, w=w: e.tensor_scalar(bl["k"][:, 0:w], bl["f"][:, 0:w], -1.0, 1.0, ALU.mult, ALU.add), reads=[tb["f"]], writes=[tb["k"]])
                if d == 0:
                    ph.dve(lambda e, w=w: e.tensor_tensor_scan(bl["bc"][:, 0:w], rmask[:, 0:w], bl["g"][:, 0:w], 0.0, ALU.mult, ALU.add),
                           reads=[tb["g"]], writes=[tb["bc"]])
                else:
                    ph.dve(lambda e, w=w: e.tensor_tensor_scan(rev(bl["bc"][:, 0:w]), rmask[:, 0:w], rev(bl["g"][:, 0:w]), 0.0, ALU.mult, ALU.add),
                           reads=[tb["g"]], writes=[tb["bc"]])
                ph.act(lambda e, w=w: e.activation(out=bl["e1"][:, 0:w], in_=bl["bc"][:, 0:w], func=AF.Exp), reads=[tb["bc"]], writes=[tb["e1"]])
                ph.act(lambda e, w=w: e.activation(out=bl["e2"][:, 0:w], in_=bl["bc"][:, 0:w], func=AF.Exp, scale=-1.0), reads=[tb["bc"]], writes=[tb["e2"]])
                ph.dve(lambda e, w=w: e.tensor_tensor(Qt[:, 0:w], bl["q"][:, 0:w], bl["e1"][:, 0:w], ALU.mult), reads=[tb["q"], tb["e1"]], writes=[tQt])
                ph.dve(lambda e, w=w: e.tensor_tensor(Kt[:, 0:w], bl["k"][:, 0:w], bl["e2"][:, 0:w], ALU.mult), reads=[tb["k"], tb["e2"]], writes=[tKt])
                eo = (CH - 1) if d == 0 else 0
                decv = bl["e1"][:, eo:w:CH]
                ph.dve(lambda e, w=w, nch=nch, decv=decv: e.tensor_tensor(
                    Kh[:, 0:w].rearrange("p (c s) -> p c s", s=CH), Kt[:, 0:w].rearrange("p (c s) -> p c s", s=CH),
                    bc_last(decv, CH), ALU.mult), reads=[tKt, tb["e1"]], writes=[tKh])
                chs = list(range(nch)) if d == 0 else list(range(nch - 1, -1, -1))
                cbase = cc
                cc += nch

                def stepA(i, chs=chs, cbase=cbase, b0=b0, d=d):
                    o = chs[i] * CH
                    gc = (b0 + o) // CH
                    p = (cbase + i) % 2
                    ph.pe(lambda e, p=p, o=o: e.matmul(psA[p][:, :], Kt[:, o:o + CH], Qt[:, o:o + CH], start=True, stop=True),
                          reads=[tKt, tQt], writes=[tpsA[p]])
                    ph.dve(lambda e, p=p, d=d: e.tensor_tensor(ATm[p][:, :], psA[p][:, :], tri[0:CH, d, 0:CH], ALU.mult),
                           reads=[tpsA[p]], writes=[tATm[p]])
                    ph.pe(lambda e, p=p, o=o: e.transpose(psT[p][:, :], Kh[:, o:o + CH], identB[:, :]), reads=[tKh], writes=[tpsT[p]])
                    ph.act(lambda e, p=p: e.copy(KhT[p][:, :], psT[p][:, :]), reads=[tpsT[p]], writes=[tKhT[p]])
                    ph.pe(lambda e, p=p, gc=gc: e.matmul(psKV[p][:, :], KhT[p][:, :], Vh[:, gc, :], start=True, stop=True),
                          reads=[tKhT[p], tV], writes=[tpsKV[p]])

                def stepBC(i, chs=chs, cbase=cbase, b0=b0, eo=eo):
                    nonlocal sp
                    o = chs[i] * CH
                    gc = (b0 + o) // CH
                    p = (cbase + i) % 2
                    s0, s1 = sp, 1 - sp
                    sp = s1
                    ph.pe(lambda e, o=o, s0=s0: e.matmul(psO[:, o:o + CH], Sb[s0][:, :], Qt[:, o:o + CH], start=True, stop=False),
                          reads=[tSb[s0], tQt], writes=[tpsO])
                    ph.pe(lambda e, o=o, p=p, gc=gc: e.matmul(psO[:, o:o + CH], Vh[:, gc, :], ATm[p][:, :], start=False, stop=True),
                          reads=[tV, tATm[p]], writes=[tpsO])
                    dsc = bl["e1"][:, o + eo:o + eo + 1]
                    ph.dve(lambda e, p=p, dsc=dsc: e.scalar_tensor_tensor(S[:, :], S[:, :], dsc, psKV[p][:, :], ALU.mult, ALU.add),
                           reads=[tS, tpsKV[p], tb["e1"]], writes=[tS])
                    ph.act(lambda e, s1=s1: e.copy(Sb[s1][:, :], S[:, :]), reads=[tS], writes=[tSb[s1]])

                for i in range(nch + 1):
                    if i < nch:
                        stepA(i)
                    if i >= 1:
                        stepBC(i - 1)
                if d == 0:
                    ph.act(lambda e, b0=b0, w=w: e.copy(of_[:, b0:b0 + w], psO[:, 0:w]), reads=[tpsO], writes=[tof])
                else:
                    ph.dve(lambda e, b0=b0, w=w: e.tensor_tensor(of_[:, b0:b0 + w], of_[:, b0:b0 + w], psO[:, 0:w], ALU.add),
                           reads=[tpsO, tof], writes=[tof])
                    ph.act(lambda e, b0=b0, w=w: e.activation(out=bl["sq"][:, 0:w], in_=of_[:, b0:b0 + w], func=AF.Square), reads=[tof], writes=[tb["sq"]])
                    ph.pe(lambda e, w=w: e.matmul(psM[:, 0:w], onesF[:, :], bl["sq"][:, 0:w], start=True, stop=True), reads=[tb["sq"]], writes=[tpsM])
                    ph.act(lambda e, w=w: e.activation(out=bl["rs"][:, 0:w], in_=psM[:, 0:w], func=AF.Sqrt, bias=self.epsb[:, :]), reads=[tpsM], writes=[tb["rs"]])
                    ph.dve(lambda e, w=w: e.reciprocal(bl["rs"][:, 0:w], bl["rs"][:, 0:w]), reads=[tb["rs"]], writes=[tb["rs"]])
                    ph.load(bl["og"][:, 0:w], dr["UBT"][1536 + h * 128:1536 + (h + 1) * 128, b0:b0 + w], writes=[tb["og"]])
                    ph.act(lambda e, w=w: e.activation(out=bl["og"][:, 0:w], in_=bl["og"][:, 0:w], func=AF.Silu), reads=[tb["og"]], writes=[tb["og"]])
                    ph.dve(lambda e, b0=b0, w=w: e.tensor_tensor(bl["t1"][:, 0:w], of_[:, b0:b0 + w], bl["rs"][:, 0:w], ALU.mult),
                           reads=[tof, tb["rs"]], writes=[tb["t1"]])
                    ph.dve(lambda e, w=w: e.scalar_tensor_tensor(yb[:, 0:w], bl["t1"][:, 0:w], par[:, ogn:ogn + 1], bl["og"][:, 0:w], ALU.mult, ALU.mult),
                           reads=[tb["t1"], tb["og"]], writes=[tyb])
                    ph.store(dr["MIXT"][512 + h * 128:512 + (h + 1) * 128, b0:b0 + w], yb[:, 0:w], reads=[tyb])
    if "DBG" in self.debug:
        ph.store(dr["DBG"][0, :, :], of_[:, :], reads=[tof])
        for i_, n_ in enumerate(["f", "g", "k", "bc", "e1", "e2", "rs", "t1", "og", "q"]):
            ph.store(dr["DBG"][1 + i_, :, 0:512], bl[n_][:, :], reads=[tb[n_]])
    ph.finish()


Prog.phase_p3 = phase_p3


def phase_p4a(self, l):
    K, dr = self.K, self.dr
    ph = Phase(K, "p4a_%d" % l)
    identB = self.identB
    gain = ph.sb("gain", [128, 10, 128], F32)
    tg = Tok()
    for hd in range(10):
        src = dr["q_norm"][l, :] if hd < 8 else dr["k_norm"][l, :]
        ph.load(gain[:, hd, :], src.partition_broadcast(128), writes=[tg])
    uc = [ph.sb("uc", [128, 1536], F32) for _ in range(2)]
    tuc = [Tok() for _ in range(2)]
    cs = [ph.sb("cs", [128, 2, 2, 2, 32], F32) for _ in range(2)]
    tcs = [Tok() for _ in range(2)]
    sq = ph.sb("sq", [128, 10, 128], F32)
    tsq = Tok()
    ss = [ph.sb("ss", [128, 10], F32) for _ in range(2)]
    tss = [Tok() for _ in range(2)]
    xn = ph.sb("xn", [128, 10, 2, 2, 32], F32)
    txn = Tok()
    t1 = ph.sb("t1", [128, 10, 2, 2, 32], F32)
    tt1 = Tok()
    t2 = ph.sb("t2", [128, 10, 2, 2, 32], F32)
    tt2 = Tok()
    qkb = [ph.sb("qkb", [128, 10, 128], BF16) for _ in range(2)]
    tqkb = [Tok() for _ in range(2)]
    vb = [ph.sb("vb", [128, 256], BF16) for _ in range(2)]
    tvb = [Tok() for _ in range(2)]
    psQ = [ph.ps("psQ", [128, 8, 128], BF16) for _ in range(2)]
    tpsQ = [Tok() for _ in range(2)]
    psK = [ph.ps("psK", [128, 2, 128], BF16) for _ in range(2)]
    tpsK = [Tok() for _ in range(2)]
    qst = [ph.sb("qst", [128, 10, 512], BF16) for _ in range(2)]
    tqst = [Tok() for _ in range(2)]
    it = 0
    for bi, (b0, w) in enumerate(BLOCKS):
        sb_ = bi % 2
        for tt in range(w // 128):
            t0 = b0 + tt * 128
            p = it % 2
            it += 1
            ph.load(uc[p][:, :], dr["UC"][t0:t0 + 128, :], writes=[tuc[p]])
            ph.load(cs[p][:, :, :, :, :], dr["rope"][t0:t0 + 128, :].rearrange("t (a b h f) -> t a b h f", a=2, b=2, h=2), writes=[tcs[p]])
            ucv = uc[p][:, 0:1280].rearrange("t (h d) -> t h d", d=128)
            ph.act(lambda e, ucv=ucv: e.activation(out=sq[:, :, :], in_=ucv, func=AF.Square), reads=[tuc[p]], writes=[tsq])
            ph.dve(lambda e, p=p: e.tensor_reduce(ss[p][:, :], sq[:, :, :], AX.X, ALU.add), reads=[tsq], writes=[tss[p]])
            ph.act(lambda e, p=p: e.activation(out=ss[p][:, :], in_=ss[p][:, :], func=AF.Sqrt, scale=1.0 / 128.0, bias=self.epsb[:, :]),
                   reads=[tss[p]], writes=[tss[p]])
            ph.dve(lambda e, p=p: e.reciprocal(ss[p][:, :], ss[p][:, :]), reads=[tss[p]], writes=[tss[p]])
            xnv = xn[:, :, :, :, :].rearrange("t h b x f -> t h (b x f)")
            ph.dve(lambda e, p=p, ucv=ucv, xnv=xnv: e.tensor_tensor(xnv, ucv, bc_last(ss[p][:, :], 128), ALU.mult),
                   reads=[tuc[p], tss[p]], writes=[txn])
            ph.dve(lambda e, xnv=xnv: e.tensor_tensor(xnv, xnv, gain[:, :, :], ALU.mult), reads=[txn, tg], writes=[txn])
            cosv = cs[p][:, 0, :, :, :].rearrange("t b x f -> t (b x f)")
            t1v = t1[:, :, :, :, :].rearrange("t h b x f -> t h (b x f)")
            ph.dve(lambda e, xnv=xnv, t1v=t1v, cosv=cosv: e.tensor_tensor(t1v, xnv, bc_mid(cosv, 10), ALU.mult),
                   reads=[txn, tcs[p]], writes=[tt1])
            for hf in range(2):
                sinv = cs[p][:, 1, :, hf, :]
                ph.dve(lambda e, hf=hf, sinv=sinv: e.tensor_tensor(t2[:, :, :, hf, :], xn[:, :, :, 1 - hf, :], bc_mid(sinv, 10), ALU.mult),
                       reads=[txn, tcs[p]], writes=[tt2])
            t2v = t2[:, :, :, :, :].rearrange("t h b x f -> t h (b x f)")
            ph.dve(lambda e, p=p, t1v=t1v, t2v=t2v: e.tensor_tensor(qkb[p][:, :, :], t1v, t2v, ALU.add), reads=[tt1, tt2], writes=[tqkb[p]])
            for hd in range(10):
                if hd < 8:
                    ph.pe(lambda e, p=p, hd=hd: e.transpose(psQ[p][:, hd, :], qkb[p][:, hd, :], identB[:, :]), reads=[tqkb[p]], writes=[tpsQ[p]])
                else:
                    ph.pe(lambda e, p=p, hd=hd: e.transpose(psK[p][:, hd - 8, :], qkb[p][:, hd, :], identB[:, :]), reads=[tqkb[p]], writes=[tpsK[p]])
            c0 = tt * 128
            ph.act(lambda e, p=p, sb_=sb_, c0=c0: e.copy(qst[sb_][:, 0:8, c0:c0 + 128], psQ[p][:, :, :]), reads=[tpsQ[p]], writes=[tqst[sb_]])
            ph.act(lambda e, p=p, sb_=sb_, c0=c0: e.copy(qst[sb_][:, 8:10, c0:c0 + 128], psK[p][:, :, :]), reads=[tpsK[p]], writes=[tqst[sb_]])
            ph.pool(lambda e, p=p: e.tensor_copy(vb[p][:, :], uc[p][:, 1280:1536]), reads=[tuc[p]], writes=[tvb[p]])
            ph.store(dr["V"][t0:t0 + 128, :], vb[p][:, :], reads=[tvb[p]])
        ph.store(dr["QT"][:, :, b0:b0 + w].rearrange("h d t -> d h t"), qst[sb_][:, 0:8, 0:w], reads=[tqst[sb_]])
        ph.store(dr["KT"][:, :, b0:b0 + w].rearrange("h d t -> d h t"), qst[sb_][:, 8:10, 0:w], reads=[tqst[sb_]])
    ph.finish()


def phase_p4b(self, l):
    K, dr = self.K, self.dr
    ph = Phase(K, "p4b_%d" % l)
    onesB = self.onesB
    KTn = ph.sb("KTn", [128, T], BF16)
    Vn = ph.sb("Vn", [128, NT, 128], BF16)
    tKT, tV = Tok(), Tok()
    Qb = [ph.sb("Qb", [128, 512], BF16) for _ in range(2)]
    tQb = [Tok() for _ in range(2)]
    Pt = [ph.sb("Pt", [128, 512], BF16) for _ in range(3)]
    tPt = [Tok() for _ in range(3)]
    psS = [ph.ps("psS", [128, 512]) for _ in range(2)]
    tpsS = [Tok() for _ in range(2)]
    psO = [ph.ps("psO", [128, 512]) for _ in range(2)]
    tpsO = [Tok() for _ in range(2)]
    psR = [ph.ps("psR", [128, 512]) for _ in range(2)]
    tpsR = [Tok() for _ in range(2)]
    rinv = [ph.sb("rinv", [128, 512], F32) for _ in range(2)]
    trinv = [Tok() for _ in range(2)]
    ob = [ph.sb("ob", [128, 512], BF16) for _ in range(2)]
    tob = [Tok() for _ in range(2)]
    nq = 0
    ns = 0
    npt = 0
    for n in range(2):
        ph.load(KTn[:, :], dr["KT"][n, :, :], writes=[tKT])
        ph.load(Vn[:, :, :], dr["V"][:, n * 128:(n + 1) * 128].rearrange("(j p) c -> p j c", p=128), writes=[tV])
        for g in range(4):
            hd = n * 4 + g
            for bi, (b0, w) in enumerate(BLOCKS):
                kts = [0, 1] if bi == 0 else list(range(NT))
                qi = nq % 2
                nq += 1
                ph.load(Qb[qi][:, 0:w], dr["QT"][hd, :, b0:b0 + w], writes=[tQb[qi]])

                def smm(kt, qi=qi, w=w):
                    nonlocal ns
                    si = ns % 2
                    ns += 1
                    ph.pe(lambda e, si=si, kt=kt, qi=qi, w=w: e.matmul(psS[si][:, 0:w], KTn[:, kt * 128:(kt + 1) * 128], Qb[qi][:, 0:w], start=True, stop=True),
                          reads=[tKT, tQb[qi]], writes=[tpsS[si]])
                    return si
                si = smm(kts[0])
                for j, kt in enumerate(kts):
                    si_next = smm(kts[j + 1]) if j + 1 < len(kts) else None
                    pi = npt % 3
                    npt += 1
                    ph.act(lambda e, si=si, pi=pi, w=w: e.activation(out=Pt[pi][:, 0:w], in_=psS[si][:, 0:w], func=AF.Exp, scale=ATT_SCALE),
                           reads=[tpsS[si]], writes=[tPt[pi]])
                    ph.pe(lambda e, qi=qi, kt=kt, pi=pi, w=w, j=j, nk=len(kts): e.matmul(psO[qi][:, 0:w], Vn[:, kt, :], Pt[pi][:, 0:w], start=(j == 0), stop=(j == nk - 1)),
                          reads=[tV, tPt[pi]], writes=[tpsO[qi]])
                    ph.pe(lambda e, qi=qi, pi=pi, w=w, j=j, nk=len(kts): e.matmul(psR[qi][:, 0:w], onesB[:, :], Pt[pi][:, 0:w], start=(j == 0), stop=(j == nk - 1)),
                          reads=[tPt[pi]], writes=[tpsR[qi]])
                    si = si_next
                ph.dve(lambda e, qi=qi, w=w: e.reciprocal(rinv[qi][:, 0:w], psR[qi][:, 0:w]), reads=[tpsR[qi]], writes=[trinv[qi]])
                ph.dve(lambda e, qi=qi, w=w: e.tensor_tensor(ob[qi][:, 0:w], psO[qi][:, 0:w], rinv[qi][:, 0:w], ALU.mult),
                       reads=[tpsO[qi], trinv[qi]], writes=[tob[qi]])
                ph.store(dr["MIXT"][1024 + hd * 128:1024 + (hd + 1) * 128, b0:b0 + w], ob[qi][:, 0:w], reads=[tob[qi]])
    ph.finish()


Prog.phase_p4a = phase_p4a
Prog.phase_p4b = phase_p4b


def load_cast_weight(ph, dst_bf, tdst, src_ap_fn, nchunks, stage, tstage, cast_engs, ctr):
    for c in range(nchunks):
        j = ctr[0] % len(stage)
        ctr[0] += 1
        src, dstv = src_ap_fn(c)
        ph.load(stage[j], src, writes=[tstage[j]]) if False else None
        yield c, j, src, dstv


def phase_p5(self, l):
    K, dr = self.K, self.dr
    ph = Phase(K, "p5_%d" % l)
    ll = l if self.n_layers > 1 else 0
    wo = ph.sb("wo", [128, KC, D], BF16)
    two = Tok()
    stg = [ph.sb("stg", [128, KC, 256], F32) for _ in range(2)]
    tstg = [Tok() for _ in range(2)]
    wsrc = dr["w_out"][ll].rearrange("(k p) n -> p k n", p=128)
    for c in range(D // 256):
        j = c % 2
        ph.load(stg[j][:, :, :], wsrc[:, :, c * 256:(c + 1) * 256], writes=[tstg[j]])
        ph.pool(lambda e, j=j, c=c: e.tensor_copy(wo[:, :, c * 256:(c + 1) * 256], stg[j][:, :, :]), reads=[tstg[j]], writes=[two])
    gbc = [ph.sb("gbc", [128, D], F32) for _ in range(2)]
    tg = Tok()
    for ty in range(2):
        ph.load(gbc[ty][:, :], dr["MODROW"][l, ty, 2 * D:3 * D].partition_broadcast(128), writes=[tg])
    mx = [ph.sb("mx", [128, KC, 512], BF16) for _ in range(2)]
    tmx = [Tok() for _ in range(2)]
    xt = [ph.sb("xt", [128, D], F32) for _ in range(2)]
    txt = [Tok() for _ in range(2)]
    tmp = [ph.sb("tmp", [128, 512], F32) for _ in range(2)]
    ttmp = [Tok() for _ in range(2)]
    pm = [ph.ps("pm", [128, 512]) for _ in range(4)]
    tpm = [Tok() for _ in range(4)]
    msrc = dr["MIXT"].rearrange("(k p) t -> p k t", p=128)
    it = 0
    nm = 0
    for bi, (b0, w) in enumerate(BLOCKS):
        mi = bi % 2
        ph.load(mx[mi][:, :, 0:w], msrc[:, :, b0:b0 + w], writes=[tmx[mi]])
        for tt in range(w // 128):
            t0 = b0 + tt * 128
            ty = 1 if t0 < NCTX else 0
            p = it % 2
            it += 1
            ph.load(xt[p][:, :], dr["X"][t0:t0 + 128, :], writes=[txt[p]])
            for dc in range(4):
                q = nm % 4
                nm += 1
                for k in range(KC):
                    ph.pe(lambda e, q=q, mi=mi, k=k, tt=tt, dc=dc: e.matmul(pm[q][:, :], mx[mi][:, k, tt * 128:(tt + 1) * 128], wo[:, k, dc * 512:(dc + 1) * 512],
                                                                             start=(k == 0), stop=(k == KC - 1)),
                          reads=[tmx[mi], two], writes=[tpm[q]])
                tp = nm % 2
                ph.dve(lambda e, q=q, tp=tp, ty=ty, dc=dc: e.tensor_tensor(tmp[tp][:, :], pm[q][:, :], gbc[ty][:, dc * 512:(dc + 1) * 512], ALU.mult),
                       reads=[tpm[q], tg], writes=[ttmp[tp]])
                ph.pool(lambda e, p=p, tp=tp, dc=dc: e.tensor_tensor(xt[p][:, dc * 512:(dc + 1) * 512], xt[p][:, dc * 512:(dc + 1) * 512], tmp[tp][:, :], ALU.add),
                        reads=[ttmp[tp], txt[p]], writes=[txt[p]])
            ph.store(dr["X"][t0:t0 + 128, :], xt[p][:, :], reads=[txt[p]])
    ph.finish()


def phase_p6a(self, l):
    K, dr = self.K, self.dr
    ph = Phase(K, "p6a_%d" % l)
    moe = (l % 2 == 1)
    identF = self.identF
    hst = ph.sb("hst", [128, KC, 512], BF16)
    thst = Tok()
    if moe:
        rt = ph.sb("rt", [128, KC, NE], F32)
        trt = Tok()
        ph.load(rt[:, :, :], dr["router"][l // 2].rearrange("(k p) e -> p k e", p=128), writes=[trt])
        h2f = ph.sb("h2f", [128, KC, 128], F32)
        th2f = Tok()
        psL = ph.ps("psL", [128, NE])
        tpsL = Tok()
        psG = ph.ps("psG", [NE, 128])
        tpsG = Tok()
        sm = {n: ph.sb(n, [128, NE], F32) for n in ("lg", "eq1", "lg2", "eq2", "gt")}
        s1 = {n: ph.sb(n, [128, 1], F32) for n in ("m1", "m2", "dm", "w1", "w2")}
        tsm = Tok()
        gst = ph.sb("gst", [NE, 512], F32)
        tgst = Tok()

    for bi, (b0, w) in enumerate(BLOCKS):
        def extra(t0, col, ty, st, b0=b0):
            if not moe:
                return
            der = self.der
            for k in range(KC):
                gsv = der[:, l, ty, 2, k:k + 1]
                shv = der[:, l, ty, 3, k:k + 1]
                src = st["pT"][k // 4][:, k % 4, :]
                ph.pool_or = None
                ph.dve(lambda e, k=k, src=src, gsv=gsv, shv=shv: e.tensor_scalar(h2f[:, k, :], src, gsv, shv, ALU.mult, ALU.add),
                       reads=[st["tpT"][k // 4]], writes=[th2f])
            for k in range(KC):
                ph.pe(lambda e, k=k: e.matmul(psL[:, :], h2f[:, k, :], rt[:, k, :], start=(k == 0), stop=(k == KC - 1)),
                      reads=[th2f, trt], writes=[tpsL])
            lg, eq1, lg2, eq2, gt = sm["lg"], sm["eq1"], sm["lg2"], sm["eq2"], sm["gt"]
            m1, m2, dm, w1, w2 = s1["m1"], s1["m2"], s1["dm"], s1["w1"], s1["w2"]
            R, W = [tsm, tpsL], [tsm]
            ph.act(lambda e: e.copy(lg[:, :], psL[:, :]), reads=R, writes=W)
            ph.dve(lambda e: e.tensor_reduce(m1[:, :], lg[:, :], AX.X, ALU.max), reads=[tsm], writes=W)
            ph.dve(lambda e: e.tensor_scalar(eq1[:, :], lg[:, :], m1[:, :], None, ALU.is_equal), reads=[tsm], writes=W)
            ph.dve(lambda e: e.scalar_tensor_tensor(lg2[:, :], eq1[:, :], -1e30, lg[:, :], ALU.mult, ALU.add), reads=[tsm], writes=W)
            ph.dve(lambda e: e.tensor_reduce(m2[:, :], lg2[:, :], AX.X, ALU.max), reads=[tsm], writes=W)
            ph.dve(lambda e: e.tensor_scalar(eq2[:, :], lg2[:, :], m2[:, :], None, ALU.is_equal), reads=[tsm], writes=W)
            ph.dve(lambda e: e.tensor_tensor(dm[:, :], m2[:, :], m1[:, :], ALU.subtract), reads=[tsm], writes=W)
            ph.act(lambda e: e.activation(out=dm[:, :], in_=dm[:, :], func=AF.Exp), reads=[tsm], writes=W)
            ph.dve(lambda e: e.tensor_scalar(w1[:, :], dm[:, :], 1.0, None, ALU.add), reads=[tsm], writes=W)
            ph.dve(lambda e: e.reciprocal(w1[:, :], w1[:, :]), reads=[tsm], writes=W)
            ph.dve(lambda e: e.tensor_tensor(w2[:, :], dm[:, :], w1[:, :], ALU.mult), reads=[tsm], writes=W)
            ph.dve(lambda e: e.tensor_scalar(gt[:, :], eq1[:, :], w1[:, :], None, ALU.mult), reads=[tsm], writes=W)
            ph.dve(lambda e: e.scalar_tensor_tensor(gt[:, :], eq2[:, :], w2[:, :], gt[:, :], ALU.mult, ALU.add), reads=[tsm], writes=W)
            ph.pe(lambda e: e.transpose(psG[:, :], gt[:, :], identF[:, :]), reads=[tsm], writes=[tpsG])
            c0 = t0 - b0
            ph.act(lambda e, c0=c0: e.copy(gst[:, c0:c0 + 128], psG[:, :]), reads=[tpsG], writes=[tgst])

        norm_tiles(self, ph, l, 1, [(b0, w)], hst, thst, f32_out=extra)
        ph.store(dr["H2T"][:, :, b0:b0 + w], hst[:, :, 0:w], reads=[thst])
        if moe:
            ph.store(dr["GT"][:, b0:b0 + w], gst[:, 0:w], reads=[tgst])
    ph.finish()


Prog.phase_p5 = phase_p5
Prog.phase_p6a = phase_p6a


def phase_p6b(self, l):
    K, dr = self.K, self.dr
    moe = (l % 2 == 1)
    jj = l // 2
    if moe:
        ne = NE if self.n_layers > 1 else 1
        w1s = [dr["moe_w1"][jj, e] for e in range(ne)]
        w3s = [dr["moe_w3"][jj, e] for e in range(ne)]
        w2s = [dr["moe_w2"][jj, e] for e in range(ne)]
    else:
        ne = 1
        w1s, w3s, w2s = [dr["ffn_w1"][jj]], [dr["ffn_w3"][jj]], [dr["ffn_w2"][jj]]
    sel = self.sel

    ph = Phase(K, "p6b1_%d" % l)
    h2g = ph.sb("h2g", [128, KC, TGMAX], BF16)
    th2g = Tok()
    FW = 256
    st1 = ph.sb("st1", [128, KC, FW], F32)
    st3 = ph.sb("st3", [128, KC, FW], F32)
    tst1, tst3 = Tok(), Tok()
    w1b = [ph.sb("w1b", [128, KC, FW], BF16) for _ in range(2)]
    w3b = [ph.sb("w3b", [128, KC, FW], BF16) for _ in range(2)]
    tw1b = [Tok() for _ in range(2)]
    tw3b = [Tok() for _ in range(2)]
    psG = [ph.ps("psG", [128, 512]) for _ in range(2)]
    psU = [ph.ps("psU", [128, 512]) for _ in range(2)]
    tpsG = [Tok() for _ in range(2)]
    tpsU = [Tok() for _ in range(2)]
    sg = [ph.sb("sg", [128, 512], F32) for _ in range(2)]
    tsg = [Tok() for _ in range(2)]
    ast = [ph.sb("ast", [128, 512], BF16) for _ in range(3)]
    tast = [Tok() for _ in range(3)]
    if moe:
        gT = ph.sb("gT", [NE, TGMAX], F32)
        tgT = Tok()
        psB = [ph.ps("psB", [128, 512]) for _ in range(2)]
        tpsB = [Tok() for _ in range(2)]
        tmp = [ph.sb("tmpa", [128, 512], F32) for _ in range(2)]
        ttmp = [Tok() for _ in range(2)]
    nw = 0
    nb = 0
    na = 0
    for grp in GROUPS:
        g0 = grp[0][0]
        gw = sum(w for _, w in grp)
        ph.load(h2g[:, :, 0:gw], dr["H2T"][:, :, g0:g0 + gw], writes=[th2g])
        if moe:
            ph.load(gT[:, 0:gw], dr["GT"][:, g0:g0 + gw], writes=[tgT])
        for ex in range(ne):
            w1src = w1s[ex].rearrange("(k p) f -> p k f", p=128)
            w3src = w3s[ex].rearrange("(k p) f -> p k f", p=128)
            for fp in range(DFF // FW):
                j = nw % 2
                nw += 1
                ph.load(st1[:, :, :], w1src[:, :, fp * FW:(fp + 1) * FW], writes=[tst1])
                ph.load(st3[:, :, :], w3src[:, :, fp * FW:(fp + 1) * FW], writes=[tst3])
                ph.pool(lambda e, j=j: e.tensor_copy(w1b[j][:, :, :], st1[:, :, :]), reads=[tst1], writes=[tw1b[j]])
                ph.pool(lambda e, j=j: e.tensor_copy(w3b[j][:, :, :], st3[:, :, :]), reads=[tst3], writes=[tw3b[j]])
                for (b0, w) in grp:
                    bidx = BLOCKS.index((b0, w))
                    col = b0 - g0
                    for m in range(FW // 128):
                        fc = fp * (FW // 128) + m
                        q = nb % 2
                        nb += 1
                        for k in range(KC):
                            ph.pe(lambda e, q=q, j=j, k=k, m=m, col=col, w=w: e.matmul(psG[q][:, 0:w], w1b[j][:, k, m * 128:(m + 1) * 128], h2g[:, k, col:col + w],
                                                                                       start=(k == 0), stop=(k == KC - 1)),
                                  reads=[tw1b[j], th2g], writes=[tpsG[q]])
                        for k in range(KC):
                            ph.pe(lambda e, q=q, j=j, k=k, m=m, col=col, w=w: e.matmul(psU[q][:, 0:w], w3b[j][:, k, m * 128:(m + 1) * 128], h2g[:, k, col:col + w],
                                                                                       start=(k == 0), stop=(k == KC - 1)),
                                  reads=[tw3b[j], th2g], writes=[tpsU[q]])
                        ph.act(lambda e, q=q, w=w: e.activation(out=sg[q][:, 0:w], in_=psG[q][:, 0:w], func=AF.Silu), reads=[tpsG[q]], writes=[tsg[q]])
                        ai = na % 3
                        na += 1
                        if not moe:
                            ph.dve(lambda e, q=q, ai=ai, w=w: e.tensor_tensor(ast[ai][:, 0:w], psU[q][:, 0:w], sg[q][:, 0:w], ALU.mult),
                                   reads=[tpsU[q], tsg[q]], writes=[tast[ai]])
                        else:
                            ph.pe(lambda e, q=q, ex=ex, col=col, w=w: e.matmul(psB[q][:, 0:w], sel[:, ex, :], gT[:, col:col + w], start=True, stop=True),
                                  reads=[tgT], writes=[tpsB[q]])
                            ph.dve(lambda e, q=q, w=w: e.tensor_tensor(tmp[q][:, 0:w], psU[q][:, 0:w], sg[q][:, 0:w], ALU.mult),
                                   reads=[tpsU[q], tsg[q]], writes=[ttmp[q]])
                            ph.dve(lambda e, q=q, ai=ai, w=w: e.tensor_tensor(ast[ai][:, 0:w], psB[q][:, 0:w], tmp[q][:, 0:w], ALU.mult),
                                   reads=[tpsB[q], ttmp[q]], writes=[tast[ai]])
                        ph.store(dr["AT%d" % ex][bidx, :, fc, 0:w], ast[ai][:, 0:w], reads=[tast[ai]])
    ph.finish()

    ph = Phase(K, "p6b2_%d" % l)
    w2b = ph.sb("w2b", [128, FC, 512], BF16)
    tw2b = Tok()
    stg = [ph.sb("stg2", [128, 4, 512], F32) for _ in range(2)]
    tstg = [Tok() for _ in range(2)]
    aT = [ph.sb("aT", [128, FC, 512], BF16) for _ in range(2)]
    taT = [Tok() for _ in range(2)]
    gbc = [ph.sb("g5", [128, D], F32) for _ in range(2)]
    tg = Tok()
    for ty in range(2):
        ph.load(gbc[ty][:, :], dr["MODROW"][l, ty, 5 * D:6 * D].partition_broadcast(128), writes=[tg])
    xq = [ph.sb("xq", [128, 512], F32) for _ in range(3)]
    txq = [Tok() for _ in range(3)]
    tmp2 = [ph.sb("tmp2", [128, 512], F32) for _ in range(2)]
    ttmp2 = [Tok() for _ in range(2)]
    pm = [ph.ps("pm2", [128, 512]) for _ in range(4)]
    tpm = [Tok() for _ in range(4)]
    xtok = {}
    ns = 0
    nblk = 0
    nx = 0
    nm = 0
    for ex in range(ne):
        w2src = w2s[ex].rearrange("(c p) d -> p c d", p=128)
        for dq in range(4):
            for c4 in range(FC // 4):
                j = ns % 2
                ns += 1
                ph.load(stg[j][:, :, :], w2src[:, c4 * 4:(c4 + 1) * 4, dq * 512:(dq + 1) * 512], writes=[tstg[j]])
                ph.pool(lambda e, j=j, c4=c4: e.tensor_copy(w2b[:, c4 * 4:(c4 + 1) * 4, :], stg[j][:, :, :]), reads=[tstg[j]], writes=[tw2b])
            for bi, (b0, w) in enumerate(BLOCKS):
                ai = nblk % 2
                nblk += 1
                ph.load(aT[ai][:, :, 0:w], dr["AT%d" % ex][bi, :, :, 0:w], writes=[taT[ai]])
                for tt in range(w // 128):
                    t0 = b0 + tt * 128
                    ty = 1 if t0 < NCTX else 0
                    q = nm % 4
                    nm += 1
                    for c in range(FC):
                        ph.pe(lambda e, q=q, ai=ai, c=c, tt=tt: e.matmul(pm[q][:, :], aT[ai][:, c, tt * 128:(tt + 1) * 128], w2b[:, c, :],
                                                                         start=(c == 0), stop=(c == FC - 1)),
                              reads=[taT[ai], tw2b], writes=[tpm[q]])
                    xi = nx % 3
                    nx += 1
                    xk = (t0, dq)
                    if xk not in xtok:
                        xtok[xk] = Tok()
                    ph.load(xq[xi][:, :], dr["X"][t0:t0 + 128, dq * 512:(dq + 1) * 512], reads=[xtok[xk]], writes=[txq[xi]])
                    tp = nm % 2
                    ph.dve(lambda e, q=q, tp=tp, ty=ty, dq=dq: e.tensor_tensor(tmp2[tp][:, :], pm[q][:, :], gbc[ty][:, dq * 512:(dq + 1) * 512], ALU.mult),
                           reads=[tpm[q], tg], writes=[ttmp2[tp]])
                    ph.dve(lambda e, xi=xi, tp=tp: e.tensor_tensor(xq[xi][:, :], xq[xi][:, :], tmp2[tp][:, :], ALU.add),
                           reads=[ttmp2[tp], txq[xi]], writes=[txq[xi]])
                    ph.store(dr["X"][t0:t0 + 128, dq * 512:(dq + 1) * 512], xq[xi][:, :], reads=[txq[xi]], writes=[xtok[xk]])
    ph.finish()


Prog.phase_p6b = phase_p6b
```

```python
import numpy as np
from contextlib import ExitStack
import concourse.bass as bass
import concourse.mybir as mybir
from concourse.bass_utils import run_bass_kernel_spmd

F32 = mybir.dt.float32
BF16 = mybir.dt.bfloat16
AF = mybir.ActivationFunctionType
ALU = mybir.AluOpType
AX = mybir.AxisListType

D = 2048
NCTX = 256
NLAT = 4096
T = NCTX + NLAT
NT = T // 128
KC = D // 128
DEPTH = 4
DFF = 5632
FC = DFF // 128
NE = 8
EPS = 1e-6
IN_W = 5120
ATT_SCALE = 128 ** -0.5

BLOCKS = [(0, 256)] + [(256 + 512 * i, 512) for i in range(8)]


class Tok:
    __slots__ = ("w", "r")

    def __init__(self):
        self.w = {}
        self.r = {}


class Op:
    __slots__ = ("eng", "fn", "deps", "dma", "sem", "val", "need", "done")

    def __init__(self, eng, fn, dma):
        self.eng = eng
        self.fn = fn
        self.dma = dma
        self.deps = []
        self.sem = None
        self.val = 0
        self.need = False
        self.done = False


ENGS = ("pe", "act", "dve", "pool", "sp")


class Kern:
    def __init__(self, nc, stack):
        self.nc = nc
        self.stack = stack
        self.esem = {e: stack.enter_context(nc.semaphore("s_" + e)) for e in ("pe", "act", "dve", "pool")}
        self.ecnt = {e: 0 for e in self.esem}
        self.dpool = {
            "sp": [stack.enter_context(nc.semaphore("d_sp%d" % i)) for i in range(32)],
            "pool": [stack.enter_context(nc.semaphore("d_pl%d" % i)) for i in range(16)],
            "act": [stack.enter_context(nc.semaphore("d_ac%d" % i)) for i in range(12)],
        }
        self.dnext = {q: 0 for q in self.dpool}
        self.duse = {}
        self.dlast = {}
        self.known = {e: {} for e in ENGS}
        self.nins = 0

    def sb(self, stack, name, shape, dt):
        return stack.enter_context(self.nc.sbuf_tensor(name, list(shape), dt))

    def ps(self, stack, name, shape, dt=F32):
        return stack.enter_context(self.nc.psum_tensor(name, list(shape), dt))


class Phase:
    def __init__(self, K, name):
        self.K = K
        self.nc = K.nc
        self.name = name
        self.stack = ExitStack()
        self.ops = {e: [] for e in ENGS}
        self.uid = 0

    def sb(self, name, shape, dt):
        self.uid += 1
        return self.K.sb(self.stack, "%s_%s%d" % (self.name, name, self.uid), shape, dt)

    def ps(self, name, shape, dt=F32):
        self.uid += 1
        return self.K.ps(self.stack, "%s_%s%d" % (self.name, name, self.uid), shape, dt)

    def rec(self, eng, fn, reads=(), writes=(), dma=False):
        op = Op(eng, fn, dma)
        key = ("d", id(op)) if dma else eng
        deps = {}
        for t in reads:
            for w in t.w.values():
                deps[id(w)] = w
        for t in writes:
            if t.r:
                for r in t.r.values():
                    deps[id(r)] = r
            for k, w in t.w.items():
                if k == key:
                    continue
                if dma and w.dma:
                    continue
                deps[id(w)] = w
        for t in reads:
            t.r[key] = op
        for t in writes:
            if t.r and not (len(t.r) == 1 and key in t.r and False):
                rr = t.r
                t.w = {}
                t.r = {}
                if key in rr and rr[key] is op:
                    pass
            t.w[key] = op
        if dma:
            K = self.K
            pool = K.dpool[eng]
            s = pool[K.dnext[eng] % len(pool)]
            K.dnext[eng] += 1
            prev = K.dlast.get(id(s))
            if prev is not None:
                deps[id(prev)] = prev
            K.duse[id(s)] = K.duse.get(id(s), 0) + 1
            op.sem = s
            op.val = 16 * K.duse[id(s)]
            K.dlast[id(s)] = op
        op.deps = [d for d in deps.values() if not d.done and d is not op]
        for d in op.deps:
            d.need = True
        self.ops[eng].append(op)
        return op

    def pe(self, fn, reads=(), writes=()):
        return self.rec("pe", fn, reads, writes)

    def act(self, fn, reads=(), writes=()):
        return self.rec("act", fn, reads, writes)

    def dve(self, fn, reads=(), writes=()):
        return self.rec("dve", fn, reads, writes)

    def pool(self, fn, reads=(), writes=()):
        return self.rec("pool", fn, reads, writes)

    def load(self, out, in_, writes=(), reads=(), q="sp", **kw):
        return self.rec(q, lambda e: e.dma_start(out=out, in_=in_, **kw), reads, writes, dma=True)

    def store(self, out, in_, reads=(), writes=(), q="pool", **kw):
        return self.rec(q, lambda e: e.dma_start(out=out, in_=in_, **kw), reads, writes, dma=True)

    def finish(self):
        K = self.K
        nc = self.nc
        for e in ("pe", "act", "dve", "pool"):
            for op in self.ops[e]:
                if not op.dma and op.need:
                    K.ecnt[e] += 1
                    op.sem = K.esem[e]
                    op.val = K.ecnt[e]

        def emit(ename, eng):
            known = K.known[ename]
            mydma = {}
            for op in self.ops[ename]:
                for d in op.deps:
                    if d.done:
                        continue
                    sid = id(d.sem)
                    if known.get(sid, 0) < d.val:
                        eng.wait_ge(d.sem, d.val)
                        known[sid] = d.val
                        K.nins += 1
                ins = op.fn(eng)
                K.nins += 1
                if op.dma:
                    ins.then_inc(op.sem, 16)
                    mydma[id(op.sem)] = (op.sem, op.val)
                elif op.need:
                    ins.then_inc(op.sem, 1)
            for sid, (s, v) in mydma.items():
                if known.get(sid, 0) < v:
                    eng.wait_ge(s, v)
                    known[sid] = v

        with nc.Block() as block:
            @block.tensor
            def _(e):
                emit("pe", e)

            @block.scalar
            def _(e):
                emit("act", e)

            @block.vector
            def _(e):
                emit("dve", e)

            @block.gpsimd
            def _(e):
                emit("pool", e)

            @block.sync
            def _(e):
                emit("sp", e)

        for e in ENGS:
            for op in self.ops[e]:
                op.done = True
        allk = {}
        for e in ("pe", "act", "dve", "pool"):
            allk[id(K.esem[e])] = K.ecnt[e]
        for q, pool in K.dpool.items():
            for s in pool:
                allk[id(s)] = 16 * K.duse.get(id(s), 0)
        for e in ENGS:
            K.known[e] = dict(allk)
        self.stack.close()


def bc_last(ap, n):
    return bass.AP(ap.tensor, ap.offset, [list(x) for x in ap.ap] + [[0, n]])


def _colvec(v):
    v = np.asarray(v, np.float32)
    return np.ascontiguousarray(v.reshape(-1, 128).T)


class PLayout:
    def __init__(self):
        self.off = {}
        self.n = 0

    def add(self, name, w):
        self.off[name] = (self.n, w)
        self.n += w


def param_layout():
    P = PLayout()
    for l in range(DEPTH):
        P.add("norm1_%d" % l, 16)
        P.add("norm2_%d" % l, 16)
        P.add("convw_%d" % l, 16)
        P.add("convb_%d" % l, 4)
        P.add("lruba_%d" % l, 8)
        P.add("lrubi_%d" % l, 8)
        P.add("lrulam_%d" % l, 8)
        P.add("gnorm_%d" % l, 1)
    P.add("lblog", 2 * DEPTH * 4)
    P.add("cT", 16)
    P.add("cctxT", 16)
    return P


PL = param_layout()


def pack_params(inp, b):
    P = np.zeros((128, PL.n), np.float32)

    def put(name, arr):
        o, w = PL.off[name]
        arr = np.asarray(arr, np.float32).reshape(128, w)
        P[:, o:o + w] = arr

    for l in range(DEPTH):
        put("norm1_%d" % l, _colvec(inp["norm1"][l]))
        put("norm2_%d" % l, _colvec(inp["norm2"][l]))
        cw = np.stack([_colvec(inp["conv_w"][l][k]) for k in range(4)], axis=1)
        put("convw_%d" % l, cw)
        put("convb_%d" % l, _colvec(inp["conv_b"][l]))
        put("lruba_%d" % l, np.stack([_colvec(inp["lru_ba"][l][d]) for d in range(2)], axis=1))
        put("lrubi_%d" % l, np.stack([_colvec(inp["lru_bi"][l][d]) for d in range(2)], axis=1))
        put("lrulam_%d" % l, np.stack([_colvec(inp["lru_lambda"][l][d]) for d in range(2)], axis=1))
        put("gnorm_%d" % l, np.asarray(inp["hgrn_gnorm"][l], np.float32).reshape(128, 1))
    lb = np.asarray(inp["hgrn_lb_logits"], np.float32)
    lbp = lb.reshape(2, DEPTH, 4, 128).transpose(3, 0, 1, 2)
    put("lblog", lbp)
    put("cT", _colvec(inp["c"][b]))
    put("cctxT", _colvec(inp["c_ctx"]))
    return P


def rope_tables_host():
    F = 32
    inv = (10000.0 ** (-np.arange(F, dtype=np.float32) / F)).astype(np.float32)
    pos = np.arange(NLAT)
    row = (pos // 64).astype(np.float32)
    col = (pos % 64).astype(np.float32)
    ar = row[:, None] * inv[None, :]
    ac = col[:, None] * inv[None, :]
    cosF = np.ones((T, 128), np.float32)
    sinS = np.zeros((T, 128), np.float32)
    cr, sr, cc, sc = np.cos(ar), np.sin(ar), np.cos(ac), np.sin(ac)
    cosF[NCTX:, 0:32] = cr
    cosF[NCTX:, 32:64] = cr
    cosF[NCTX:, 64:96] = cc
    cosF[NCTX:, 96:128] = cc
    sinS[NCTX:, 0:32] = -sr
    sinS[NCTX:, 32:64] = sr
    sinS[NCTX:, 64:96] = -sc
    sinS[NCTX:, 96:128] = sc
    return np.concatenate([cosF, sinS], axis=1).astype(np.float32)


def bc_mid(ap, n):
    a = [list(x) for x in ap.ap]
    return bass.AP(ap.tensor, ap.offset, [a[0], [0, n]] + a[1:])


class Prog:
    def __init__(self, n_layers=DEPTH, debug=(), stop_after=None):
        self.n_layers = n_layers
        self.debug = set(debug)
        self.stop_after = stop_after
        self.nc = bass.Bass("TRN2", target_bir_lowering=False)
        self.stack = ExitStack()
        self.K = Kern(self.nc, self.stack)
        self.dr = {}

    def din(self, name, shape, dt=F32):
        self.dr[name] = self.nc.dram_tensor(name, list(shape), dt, kind="ExternalInput").ap()
        return self.dr[name]

    def dscratch(self, name, shape, dt=F32):
        kind = "ExternalOutput" if name in self.debug else "Internal"
        self.dr[name] = self.nc.dram_tensor(name, list(shape), dt, kind=kind).ap()
        return self.dr[name]

    def declare(self):
        L = DEPTH if self.n_layers > 1 else 1
        NEd = NE if self.n_layers > 1 else 1
        self.din("x", [NLAT, D])
        self.din("ctx", [NCTX, D])
        self.din("params", [128, PL.n])
        self.din("rope", [T, 256])
        self.din("w_mod", [L, D, 6 * D])
        self.din("b_mod", [L, 6 * D])
        self.din("w_in", [L, D, IN_W])
        self.din("w_out", [L, D, D])
        self.din("lru_wa", [L, 2, 4, 128, 128])
        self.din("lru_wi", [L, 2, 4, 128, 128])
        self.din("q_norm", [L, 128])
        self.din("k_norm", [L, 128])
        self.din("ffn_w1", [2, D, DFF])
        self.din("ffn_w3", [2, D, DFF])
        self.din("ffn_w2", [2, DFF, D])
        self.din("router", [2, D, NE])
        self.din("moe_w1", [2, NEd, D, DFF])
        self.din("moe_w3", [2, NEd, D, DFF])
        self.din("moe_w2", [2, NEd, DFF, D])
        self.dr["out"] = self.nc.dram_tensor("out", [NLAT, D], F32, kind="ExternalOutput").ap()
        self.dscratch("X", [T, D])
        self.dscratch("MODROW", [DEPTH, 2, 6 * D])
        self.dscratch("UAT", [1024, T])
        self.dscratch("UBT", [2048, T])
        self.dscratch("UBI", [T, 512], BF16)
        self.dscratch("UC", [T, 1536])
        self.dscratch("MIXT", [2048, T], BF16)
        self.dscratch("QT", [8, 128, T], BF16)
        self.dscratch("KT", [2, 128, T], BF16)
        self.dscratch("V", [T, 256], BF16)
        self.dscratch("H2T", [128, KC, T], BF16)
        self.dscratch("GT", [NE, T])
        if "DBG" in self.debug:
            self.dscratch("DBG", [12, 128, T])
        for e_ in range(NE):
            self.dscratch("AT%d" % e_, [len(BLOCKS), 128, FC, 512], BF16)

    def persistent(self):
        K, st = self.K, self.stack
        self.par = K.sb(st, "par", [128, PL.n], F32)
        self.identF = K.sb(st, "identF", [128, 128], F32)
        self.identB = K.sb(st, "identB", [128, 128], BF16)
        self.onesF = K.sb(st, "onesF", [128, 128], F32)
        self.onesB = K.sb(st, "onesB", [128, 128], BF16)
        self.tri = K.sb(st, "tri", [64, 2, 64], F32)
        self.sel = K.sb(st, "sel", [8, NE, 128], F32)
        self.rmask = K.sb(st, "rmask", [128, 512], F32)
        self.modT = K.sb(st, "modT", [128, DEPTH, 96, 2], F32)
        self.der = K.sb(st, "der", [128, DEPTH, 2, 4, 16], F32)
        self.sp8 = K.sb(st, "sp8", [128, DEPTH, 8], F32)
        self.sp16 = K.sb(st, "sp16", [128, DEPTH, 8], F32)
        self.low = K.sb(st, "low", [128, 2, DEPTH, 4], F32)
        self.oml = K.sb(st, "oml", [128, 2, DEPTH, 4], F32)
        self.sc = K.sb(st, "sc", [128, 16, 2], F32)
        self.epsb = K.sb(st, "epsb", [128, 1], F32)

    def pv(self, name, l=None):
        key = name if l is None else "%s_%d" % (name, l)
        o, w = PL.off[key]
        return o, w

    def prologue(self):
        K, nc, dr = self.K, self.nc, self.dr
        ph = Phase(K, "pro")
        tk = Tok()
        par = self.par
        ph.load(par[:, :], dr["params"][:, :], writes=[tk])
        ph.load(dr["X"][0:NCTX, :], dr["ctx"][:, :])
        for i in range(4):
            ph.load(dr["X"][NCTX + 1024 * i:NCTX + 1024 * (i + 1), :], dr["x"][1024 * i:1024 * (i + 1), :])
        tc_ = Tok()
        onesF, onesB, identF, identB = self.onesF, self.onesB, self.identF, self.identB
        tmp1 = ph.sb("tmp1", [128, 128], F32)
        ph.pool(lambda e: e.memset(tmp1[:, :], 1.0), writes=[tc_])
        ph.pool(lambda e: e.memset(onesF[:, :], 1.0 / 128.0), writes=[tc_])
        ph.pool(lambda e: e.memset(onesB[:, :], 1.0), writes=[tc_])
        tid = Tok()
        ph.pool(lambda e: e.affine_select(identF[:, :], tmp1[:, :], [[-1, 128]], ALU.is_equal, 0.0,
                                          base=0, channel_multiplier=1), reads=[tc_], writes=[tid])
        ph.pool(lambda e: e.tensor_copy(identB[:, :], identF[:, :]), reads=[tid], writes=[tc_])
        tri = self.tri
        ph.pool(lambda e: e.affine_select(tri[:, 0, :], tmp1[0:64, 0:64], [[1, 64]], ALU.is_ge, 0.0,
                                          base=0, channel_multiplier=-1), reads=[tc_], writes=[tid])
        ph.pool(lambda e: e.affine_select(tri[:, 1, :], tmp1[0:64, 0:64], [[-1, 64]], ALU.is_ge, 0.0,
                                          base=0, channel_multiplier=1), reads=[tc_], writes=[tid])
        sel = self.sel
        tmp8 = ph.sb("tmp8", [8, NE, 128], F32)
        ph.pool(lambda e: e.memset(tmp8[:, :, :], 1.0), writes=[tc_])
        ph.pool(lambda e: e.affine_select(sel[:, :, :], tmp8[:, :, :], [[-1, NE], [0, 128]], ALU.is_equal, 0.0,
                                          base=0, channel_multiplier=1), reads=[tc_], writes=[tid])
        rmask = self.rmask
        epsb = self.epsb
        ph.pool(lambda e: e.memset(epsb[:, :], EPS), writes=[tc_])
        ph.pool(lambda e: e.memset(rmask[:, :], 1.0), writes=[tc_])
        ph.pool(lambda e: e.memset(rmask[:, 0:512:16], 0.0), reads=[tc_], writes=[tid])

        sc = self.sc
        oc, _ = self.pv("cT")
        occ, _ = self.pv("cctxT")
        tsc = Tok()
        ph.act(lambda e: e.activation(out=sc[:, :, 0], in_=par[:, oc:oc + 16], func=AF.Silu), reads=[tk], writes=[tsc])
        ph.act(lambda e: e.activation(out=sc[:, :, 1], in_=par[:, occ:occ + 16], func=AF.Silu), reads=[tk], writes=[tsc])

        modT = self.modT
        tmod = Tok()
        NB = 2
        wm = [ph.sb("wm", [128, KC, 512], F32) for _ in range(NB)]
        twm = [Tok() for _ in range(NB)]
        bm = [ph.sb("bm", [2, 512], F32) for _ in range(NB)]
        tbm = [Tok() for _ in range(NB)]
        row = [ph.sb("row", [2, 512], F32) for _ in range(NB)]
        trow = [Tok() for _ in range(NB)]
        psr = [ph.ps("psr", [2, 512]) for _ in range(NB)]
        tpsr = [Tok() for _ in range(NB)]
        pst = [ph.ps("pst", [128, 4, 2]) for _ in range(NB)]
        tpst = [Tok() for _ in range(NB)]
        it = 0
        for l in range(self.n_layers):
            wsrc = dr["w_mod"][l].rearrange("(k p) n -> p k n", p=128)
            for j in range(24):
                b = it % NB
                it += 1
                ph.load(wm[b][:, :, :], wsrc[:, :, j * 512:(j + 1) * 512], writes=[twm[b]])
                ph.load(bm[b][:, :], dr["b_mod"][l, j * 512:(j + 1) * 512].partition_broadcast(2), writes=[tbm[b]])
                for k in range(KC):
                    ph.pe(lambda e, b=b, k=k: e.matmul(psr[b][:, :], sc[:, k, :], wm[b][:, k, :],
                                                       start=(k == 0), stop=(k == KC - 1)),
                          reads=[tsc, twm[b]], writes=[tpsr[b]])
                ph.dve(lambda e, b=b: e.tensor_tensor(row[b][:, :], psr[b][:, :], bm[b][:, :], ALU.add),
                       reads=[tpsr[b], tbm[b]], writes=[trow[b]])
                ph.store(dr["MODROW"][l, :, j * 512:(j + 1) * 512], row[b][:, :], reads=[trow[b]])
                for i in range(4):
                    ph.pe(lambda e, b=b, i=i: e.transpose(pst[b][:, i, :], row[b][:, i * 128:(i + 1) * 128],
                                                          identF[0:2, 0:2]),
                          reads=[trow[b], tid], writes=[tpst[b]])
                ph.act(lambda e, b=b, l=l, j=j: e.copy(modT[:, l, j * 4:(j + 1) * 4, :], pst[b][:, :, :]),
                       reads=[tpst[b]], writes=[tmod])
        der = self.der
        tder = Tok()
        for l in range(self.n_layers):
            o1, _ = self.pv("norm1", l)
            o2, _ = self.pv("norm2", l)
            for ty in range(2):
                ph.dve(lambda e, l=l, ty=ty: e.tensor_scalar(der[:, l, ty, 0, :], modT[:, l, 16:32, ty], 1.0, None,
                                                              ALU.add), reads=[tmod], writes=[tder])
                ph.dve(lambda e, l=l, ty=ty, o1=o1: e.tensor_tensor(der[:, l, ty, 0, :], der[:, l, ty, 0, :],
                                                                     par[:, o1:o1 + 16], ALU.mult),
                       reads=[tder, tk], writes=[tder])
                ph.dve(lambda e, l=l, ty=ty: e.tensor_copy(der[:, l, ty, 1, :], modT[:, l, 0:16, ty]),
                       reads=[tmod], writes=[tder])
                ph.dve(lambda e, l=l, ty=ty: e.tensor_scalar(der[:, l, ty, 2, :], modT[:, l, 64:80, ty], 1.0, None,
                                                              ALU.add), reads=[tmod], writes=[tder])
                ph.dve(lambda e, l=l, ty=ty, o2=o2: e.tensor_tensor(der[:, l, ty, 2, :], der[:, l, ty, 2, :],
                                                                     par[:, o2:o2 + 16], ALU.mult),
                       reads=[tder, tk], writes=[tder])
                ph.dve(lambda e, l=l, ty=ty: e.tensor_copy(der[:, l, ty, 3, :], modT[:, l, 48:64, ty]),
                       reads=[tmod], writes=[tder])
        sp8, sp16 = self.sp8, self.sp16
        tsp = Tok()
        for l in range(self.n_layers):
            ol, _ = self.pv("lrulam", l)
            ph.act(lambda e, l=l, ol=ol: e.activation(out=sp8[:, l, :], in_=par[:, ol:ol + 8], func=AF.Exp, scale=-1.0),
                   reads=[tk], writes=[tsp])
            ph.act(lambda e, l=l: e.activation(out=sp8[:, l, :], in_=sp8[:, l, :], func=AF.Ln, bias=1.0),
                   reads=[tsp], writes=[tsp])
            ph.dve(lambda e, l=l: e.tensor_scalar(sp16[:, l, :], sp8[:, l, :], -16.0, None, ALU.mult),
                   reads=[tsp], writes=[tsp])
            ph.dve(lambda e, l=l: e.tensor_scalar(sp8[:, l, :], sp8[:, l, :], -8.0, None, ALU.mult),
                   reads=[tsp], writes=[tsp])
        low, oml = self.low, self.oml
        olb, _ = self.pv("lblog")
        ex = ph.sb("ex", [128, 2, DEPTH, 4], F32)
        mx = ph.sb("mx", [128, 2, 4], F32)
        sm = ph.sb("sm", [128, 2, 4], F32)
        tl = Tok()
        lbv = par[:, olb:olb + 2 * DEPTH * 4].rearrange("p (d l h) -> p d l h", d=2, l=DEPTH)
        ph.dve(lambda e: e.tensor_tensor(mx[:, :, :], lbv[:, :, 0, :], lbv[:, :, 1, :], ALU.max), reads=[tk], writes=[tl])
        for l in range(2, DEPTH):
            ph.dve(lambda e, l=l: e.tensor_tensor(mx[:, :, :], mx[:, :, :], lbv[:, :, l, :], ALU.max), reads=[tl, tk], writes=[tl])
        for l in range(DEPTH):
            ph.dve(lambda e, l=l: e.tensor_tensor(ex[:, :, l, :], lbv[:, :, l, :], mx[:, :, :], ALU.subtract),
                   reads=[tl, tk], writes=[tl])
        ph.act(lambda e: e.activation(out=ex[:, :, :, :], in_=ex[:, :, :, :], func=AF.Exp), reads=[tl], writes=[tl])
        ph.dve(lambda e: e.tensor_tensor(sm[:, :, :], ex[:, :, 0, :], ex[:, :, 1, :], ALU.add), reads=[tl], writes=[tl])
        for l in range(2, DEPTH):
            ph.dve(lambda e, l=l: e.tensor_tensor(sm[:, :, :], sm[:, :, :], ex[:, :, l, :], ALU.add), reads=[tl], writes=[tl])
        ph.dve(lambda e: e.reciprocal(sm[:, :, :], sm[:, :, :]), reads=[tl], writes=[tl])
        ph.dve(lambda e: e.memset(low[:, :, 0, :], 0.0), writes=[tl])
        for l in range(1, DEPTH):
            ph.dve(lambda e, l=l: e.tensor_tensor(ex[:, :, l, :], ex[:, :, l, :], sm[:, :, :], ALU.mult), reads=[tl], writes=[tl])
            ph.dve(lambda e, l=l: e.tensor_tensor(low[:, :, l, :], low[:, :, l - 1, :], ex[:, :, l, :], ALU.add),
                   reads=[tl], writes=[tl])
        ph.dve(lambda e: e.tensor_scalar(oml[:, :, :, :], low[:, :, :, :], -1.0, 1.0, ALU.mult, ALU.add), reads=[tl], writes=[tl])
        ph.finish()


SHARED = ["w_mod", "b_mod", "w_in", "w_out", "lru_wa", "lru_wi", "q_norm", "k_norm", "ffn_w1", "ffn_w3", "ffn_w2",
          "router", "moe_w1", "moe_w3", "moe_w2"]


def make_in_maps(inp, cores, small=False):
    rope = rope_tables_host()
    if small:
        shared = {}
        for k in SHARED:
            a = inp[k]
            if k.startswith("moe_"):
                a = a[:, 0:1]
            elif k in ("w_mod", "b_mod", "w_in", "w_out", "lru_wa", "lru_wi", "q_norm", "k_norm"):
                a = a[0:1]
            shared[k] = np.ascontiguousarray(np.asarray(a, np.float32))
    else:
        shared = {k: np.ascontiguousarray(np.asarray(inp[k], np.float32)) for k in SHARED}
    maps = []
    for b in cores:
        m = dict(shared)
        m["x"] = np.ascontiguousarray(np.asarray(inp["x"][b], np.float32))
        m["ctx"] = np.ascontiguousarray(np.asarray(inp["ctx"][b], np.float32))
        m["params"] = pack_params(inp, b)
        m["rope"] = rope
        maps.append(m)
    return maps


def build_program(n_layers=DEPTH, debug=(), stop_after=None):
    P = Prog(n_layers, debug, stop_after)
    P.declare()
    P.persistent()
    P.build()
    return P


def kernel(**inputs):
    P = build_program()
    maps = make_in_maps(inputs, list(range(8)))
    res = run_bass_kernel_spmd(P.nc, maps, core_ids=list(range(8)))
    out = np.stack([np.asarray(r["out"], np.float32) for r in res.results], axis=0)
    return out


GROUPS = [BLOCKS[0:5], BLOCKS[5:9]]
TGMAX = 2304


def _build(self):
    self.prologue()
    if self.stop_after == "pro":
        return self.epilogue(False)
    for l in range(self.n_layers):
        for name, fn in (("p1", self.phase_p1), ("p2", self.phase_p2), ("p3", self.phase_p3),
                         ("p4a", self.phase_p4a), ("p4b", self.phase_p4b), ("p5", self.phase_p5),
                         ("p6a", self.phase_p6a), ("p6b", self.phase_p6b)):
            fn(l)
            if self.stop_after == "%s_%d" % (name, l):
                return self.epilogue(False)
    return self.epilogue(True)


def _epilogue(self, full):
    K, dr = self.K, self.dr
    ph = Phase(K, "epi")
    for i in range(4):
        ph.load(dr["out"][1024 * i:1024 * (i + 1), :], dr["X"][NCTX + 1024 * i:NCTX + 1024 * (i + 1), :])
    ph.finish()
    self.stack.close()


def norm_tiles(self, ph, l, which, grp_blocks, hT, thT, f32_out=None):
    K, dr = self.K, self.dr
    der, identF = self.der, self.identF
    st = ph._norm_state if hasattr(ph, "_norm_state") else None
    if st is None:
        st = {}
        st["xt"] = [ph.sb("xt", [128, D], F32) for _ in range(2)]
        st["txt"] = [Tok() for _ in range(2)]
        st["xn"] = [ph.sb("xn", [128, D], F32) for _ in range(2)]
        st["txn"] = [Tok() for _ in range(2)]
        st["junk"] = ph.sb("junk", [128, D], F32)
        st["tjunk"] = Tok()
        st["ss"] = [ph.sb("ss", [128, 1], F32) for _ in range(2)]
        st["rs"] = [ph.sb("rs", [128, 1], F32) for _ in range(2)]
        st["tss"] = [Tok() for _ in range(2)]
        st["trs"] = [Tok() for _ in range(2)]
        st["pT"] = [ph.ps("pT", [128, 4, 128]) for _ in range(4)]
        st["tpT"] = [Tok() for _ in range(4)]
        st["i"] = 0
        ph._norm_state = st
    g0 = grp_blocks[0][0]
    for (b0, w) in grp_blocks:
        for tt in range(w // 128):
            t0 = b0 + tt * 128
            ty = 1 if t0 < NCTX else 0
            col = t0 - g0
            i = st["i"]
            st["i"] += 1
            p = i % 2
            xt, txt, xn, txn = st["xt"][p], st["txt"][p], st["xn"][p], st["txn"][p]
            ss, rs, tss, trs = st["ss"][p], st["rs"][p], st["tss"][p], st["trs"][p]
            junk, tjunk = st["junk"], st["tjunk"]
            ph.load(xt[:, :], dr["X"][t0:t0 + 128, :], writes=[txt])
            ph.act(lambda e, xt=xt, ss=ss: e.activation(out=junk[:, :], in_=xt[:, :], func=AF.Square, accum_out=ss[:, :]),
                   reads=[txt], writes=[tjunk, tss])
            ph.act(lambda e, ss=ss, rs=rs: e.activation(out=rs[:, :], in_=ss[:, :], func=AF.Sqrt, scale=1.0 / D, bias=self.epsb[:, :]),
                   reads=[tss], writes=[trs])
            ph.dve(lambda e, rs=rs: e.reciprocal(rs[:, :], rs[:, :]), reads=[trs], writes=[trs])
            ph.dve(lambda e, xn=xn, xt=xt, rs=rs: e.tensor_scalar(xn[:, :], xt[:, :], rs[:, :], None, ALU.mult),
                   reads=[txt, trs], writes=[txn])
            for k in range(KC):
                ph.pe(lambda e, k=k, xn=xn: e.transpose(st["pT"][k // 4][:, k % 4, :], xn[:, k * 128:(k + 1) * 128], identF[:, :]),
                      reads=[txn], writes=[st["tpT"][k // 4]])
            for k in range(KC):
                gsv = der[:, l, ty, 2 * which, k:k + 1]
                shv = der[:, l, ty, 2 * which + 1, k:k + 1]
                src = st["pT"][k // 4][:, k % 4, :]
                dst = hT[:, k, col:col + 128]
                if k % 2 == 0:
                    ph.dve(lambda e, dst=dst, src=src, gsv=gsv, shv=shv: e.tensor_scalar(dst, src, gsv, shv, ALU.mult, ALU.add),
                           reads=[st["tpT"][k // 4]], writes=[thT])
                else:
                    ph.act(lambda e, dst=dst, src=src, gsv=gsv, shv=shv: e.activation(out=dst, in_=src, func=AF.Identity, scale=gsv, bias=shv),
                           reads=[st["tpT"][k // 4]], writes=[thT])
            if f32_out is not None:
                f32_out(t0, col, ty, st)


def phase_p1(self, l):
    K, dr = self.K, self.dr
    ph = Phase(K, "p1_%d" % l)
    hT = ph.sb("hT", [128, KC, TGMAX], BF16)
    thT = Tok()
    CW = 256
    wst = [ph.sb("wst", [128, KC, CW], F32) for _ in range(2)]
    twst = [Tok() for _ in range(2)]
    wb = [ph.sb("wb", [128, KC, CW], BF16) for _ in range(2)]
    twb = [Tok() for _ in range(2)]
    pm = [ph.ps("pm", [128, 512]) for _ in range(4)]
    tpm = [Tok() for _ in range(4)]
    ost = [ph.sb("ost", [128, 512], F32) for _ in range(4)]
    tost = [Tok() for _ in range(4)]
    ostb = [ph.sb("ostb", [128, 256], BF16) for _ in range(2)]
    tostb = [Tok() for _ in range(2)]
    wsrc = dr["w_in"][l].rearrange("(k p) n -> p k n", p=128)
    cnt = {"w": 0, "m": 0, "b": 0}
    for grp in GROUPS:
        g0 = grp[0][0]
        norm_tiles(self, ph, l, 0, grp, hT, thT)
        for cg in range(IN_W // CW):
            c0 = cg * CW
            j = cnt["w"] % 2
            cnt["w"] += 1
            ph.load(wst[j][:, :, :], wsrc[:, :, c0:c0 + CW], writes=[twst[j]])
            ph.pool(lambda e, j=j: e.tensor_copy(wb[j][:, :, :], wst[j][:, :, :]), reads=[twst[j]], writes=[twb[j]])
            tokmajor = (2560 <= c0 < 3072) or (c0 >= 3584)
            if not tokmajor:
                if c0 < 1024:
                    dst, r0 = dr["UAT"], c0
                elif c0 < 2560:
                    dst, r0 = dr["UBT"], c0 - 1024
                else:
                    dst, r0 = dr["UBT"], c0 - 3072 + 1536
                for (b0, w) in grp:
                    col = b0 - g0
                    for m in range(CW // 128):
                        q = cnt["m"] % 4
                        cnt["m"] += 1
                        for k in range(KC):
                            ph.pe(lambda e, q=q, j=j, k=k, m=m, col=col, w=w: e.matmul(
                                pm[q][:, 0:w], wb[j][:, k, m * 128:(m + 1) * 128], hT[:, k, col:col + w],
                                start=(k == 0), stop=(k == KC - 1)), reads=[twb[j], thT], writes=[tpm[q]])
                        if q % 2 == 0:
                            ph.act(lambda e, q=q, w=w: e.copy(ost[q][:, 0:w], pm[q][:, 0:w]), reads=[tpm[q]], writes=[tost[q]])
                        else:
                            ph.dve(lambda e, q=q, w=w: e.tensor_copy(ost[q][:, 0:w], pm[q][:, 0:w]), reads=[tpm[q]], writes=[tost[q]])
                        ph.store(dst[r0 + m * 128:r0 + (m + 1) * 128, b0:b0 + w], ost[q][:, 0:w], reads=[tost[q]])
            else:
                for (b0, w) in grp:
                    for tt in range(w // 128):
                        t0 = b0 + tt * 128
                        col = t0 - g0
                        q = cnt["m"] % 4
                        cnt["m"] += 1
                        for k in range(KC):
                            ph.pe(lambda e, q=q, j=j, k=k, col=col: e.matmul(
                                pm[q][:, 0:CW], hT[:, k, col:col + 128], wb[j][:, k, :],
                                start=(k == 0), stop=(k == KC - 1)), reads=[twb[j], thT], writes=[tpm[q]])
                        if c0 < 3072:
                            bi = cnt["b"] % 2
                            cnt["b"] += 1
                            ph.act(lambda e, q=q, bi=bi: e.copy(ostb[bi][:, :], pm[q][:, 0:CW]), reads=[tpm[q]], writes=[tostb[bi]])
                            ph.store(dr["UBI"][t0:t0 + 128, c0 - 2560:c0 - 2560 + CW], ostb[bi][:, :], reads=[tostb[bi]])
                        else:
                            if q % 2 == 0:
                                ph.act(lambda e, q=q: e.copy(ost[q][:, 0:CW], pm[q][:, 0:CW]), reads=[tpm[q]], writes=[tost[q]])
                            else:
                                ph.dve(lambda e, q=q: e.tensor_copy(ost[q][:, 0:CW], pm[q][:, 0:CW]), reads=[tpm[q]], writes=[tost[q]])
                            ph.store(dr["UC"][t0:t0 + 128, c0 - 3584:c0 - 3584 + CW], ost[q][:, 0:CW], reads=[tost[q]])
    ph.finish()


def _stub(self, l):
    pass


for _n in ("phase_p2", "phase_p3", "phase_p4a", "phase_p4b", "phase_p5", "phase_p6a", "phase_p6b"):
    setattr(Prog, _n, _stub)
Prog.build = _build
Prog.epilogue = _epilogue
Prog.phase_p1 = phase_p1


def rev(ap):
    return ap[:, ::-1]


def phase_p2(self, l):
    K, dr, par = self.K, self.dr, self.par
    ph = Phase(K, "p2_%d" % l)
    names = ["yb", "xb", "xc", "r", "ii", "a", "m", "hf", "hb"]
    tl = {n: ph.sb(n, [128, T], F32) for n in names}
    tk = {n: Tok() for n in names}
    ob = ph.sb("ob", [128, T], BF16)
    tob = Tok()
    wa = ph.sb("wa", [128, 2, 4, 128], F32)
    wi = ph.sb("wi", [128, 2, 4, 128], F32)
    tw = Tok()
    ph.load(wa[:, :, :, :], dr["lru_wa"][l].rearrange("d n k j -> k d n j"), writes=[tw])
    ph.load(wi[:, :, :, :], dr["lru_wi"][l].rearrange("d n k j -> k d n j"), writes=[tw])
    pg = [ph.ps("pg", [128, 512]) for _ in range(4)]
    tpg = [Tok() for _ in range(4)]
    ocw, _ = self.pv("convw", l)
    ocb, _ = self.pv("convb", l)
    oba, _ = self.pv("lruba", l)
    obi, _ = self.pv("lrubi", l)
    sp8, sp16 = self.sp8, self.sp16
    yb, xb, xc, r, ii, a, m, hf, hb = [tl[n] for n in names]
    cnt = 0
    for n in range(4):
        ph.load(yb[:, :], dr["UAT"][n * 128:(n + 1) * 128, :], writes=[tk["yb"]])
        ph.load(xb[:, :], dr["UAT"][512 + n * 128:512 + (n + 1) * 128, :], writes=[tk["xb"]])
        cw = lambda k, n=n: par[:, ocw + k * 4 + n:ocw + k * 4 + n + 1]
        cb = par[:, ocb + n:ocb + n + 1]
        ph.dve(lambda e, cw=cw, cb=cb: e.tensor_scalar(xc[:, :], xb[:, :], cw(2), cb, ALU.mult, ALU.add),
               reads=[tk["xb"]], writes=[tk["xc"]])
        for k in (0, 1, 3):
            o = k - 2
            for (s0, s1) in ((0, NCTX), (NCTX, T)):
                lo = max(s0, s0 - o)
                hi = min(s1, s1 - o)
                ph.dve(lambda e, cw=cw, k=k, lo=lo, hi=hi, o=o: e.scalar_tensor_tensor(
                    xc[:, lo:hi], xb[:, lo + o:hi + o], cw(k), xc[:, lo:hi], ALU.mult, ALU.add),
                    reads=[tk["xb"], tk["xc"]], writes=[tk["xc"]])
        for d in range(2):
            ba = par[:, oba + d * 4 + n:oba + d * 4 + n + 1]
            bi = par[:, obi + d * 4 + n:obi + d * 4 + n + 1]
            for (b0, w) in BLOCKS:
                q = cnt % 4
                cnt += 1
                ph.pe(lambda e, q=q, d=d, n=n, b0=b0, w=w: e.matmul(pg[q][:, 0:w], wa[:, d, n, :], xc[:, b0:b0 + w], start=True, stop=True),
                      reads=[tw, tk["xc"]], writes=[tpg[q]])
                ph.act(lambda e, q=q, b0=b0, w=w, ba=ba: e.activation(out=r[:, b0:b0 + w], in_=pg[q][:, 0:w], func=AF.Sigmoid, bias=ba),
                       reads=[tpg[q]], writes=[tk["r"]])
                q = cnt % 4
                cnt += 1
                ph.pe(lambda e, q=q, d=d, n=n, b0=b0, w=w: e.matmul(pg[q][:, 0:w], wi[:, d, n, :], xc[:, b0:b0 + w], start=True, stop=True),
                      reads=[tw, tk["xc"]], writes=[tpg[q]])
                ph.act(lambda e, q=q, b0=b0, w=w, bi=bi: e.activation(out=ii[:, b0:b0 + w], in_=pg[q][:, 0:w], func=AF.Sigmoid, bias=bi),
                       reads=[tpg[q]], writes=[tk["ii"]])
            s8 = sp8[:, l, d * 4 + n:d * 4 + n + 1]
            s16 = sp16[:, l, d * 4 + n:d * 4 + n + 1]
            ph.act(lambda e, s8=s8: e.activation(out=a[:, :], in_=r[:, :], func=AF.Exp, scale=s8), reads=[tk["r"]], writes=[tk["a"]])
            ph.act(lambda e, s16=s16: e.activation(out=m[:, :], in_=r[:, :], func=AF.Exp, scale=s16), reads=[tk["r"]], writes=[tk["m"]])
            ph.dve(lambda e: e.tensor_scalar(m[:, :], m[:, :], -1.0, 1.0, ALU.mult, ALU.add), reads=[tk["m"]], writes=[tk["m"]])
            ph.act(lambda e: e.activation(out=m[:, :], in_=m[:, :], func=AF.Sqrt), reads=[tk["m"]], writes=[tk["m"]])
            ph.dve(lambda e: e.tensor_tensor(ii[:, :], ii[:, :], xc[:, :], ALU.mult), reads=[tk["ii"], tk["xc"]], writes=[tk["ii"]])
            ph.dve(lambda e: e.tensor_tensor(ii[:, :], ii[:, :], m[:, :], ALU.mult), reads=[tk["ii"], tk["m"]], writes=[tk["ii"]])
            if d == 0:
                ph.dve(lambda e: e.tensor_tensor_scan(hf[:, :], a[:, :], ii[:, :], 0.0, ALU.mult, ALU.add),
                       reads=[tk["a"], tk["ii"]], writes=[tk["hf"]])
            else:
                ph.dve(lambda e: e.tensor_tensor_scan(rev(hb[:, 0:NCTX]), rev(a[:, 0:NCTX]), rev(ii[:, 0:NCTX]), 0.0, ALU.mult, ALU.add),
                       reads=[tk["a"], tk["ii"]], writes=[tk["hb"]])
                ph.dve(lambda e: e.tensor_tensor_scan(rev(hb[:, NCTX:T]), rev(a[:, NCTX:T]), rev(ii[:, NCTX:T]), hb[:, 0:1], ALU.mult, ALU.add),
                       reads=[tk["a"], tk["ii"], tk["hb"]], writes=[tk["hb"]])
        ph.dve(lambda e: e.tensor_tensor(r[:, :], yb[:, :], yb[:, :], ALU.mult), reads=[tk["yb"], tk["r"]], writes=[tk["r"]])
        ph.dve(lambda e: e.tensor_scalar(r[:, :], r[:, :], 0.044715, 1.0, ALU.mult, ALU.add), reads=[tk["r"]], writes=[tk["r"]])
        ph.dve(lambda e: e.tensor_tensor(r[:, :], r[:, :], yb[:, :], ALU.mult), reads=[tk["r"], tk["yb"]], writes=[tk["r"]])
        ph.act(lambda e: e.activation(out=r[:, :], in_=r[:, :], func=AF.Sigmoid, scale=1.5957691216), reads=[tk["r"]], writes=[tk["r"]])
        ph.dve(lambda e: e.tensor_tensor(r[:, :], r[:, :], yb[:, :], ALU.mult), reads=[tk["r"], tk["yb"]], writes=[tk["r"]])
        ph.dve(lambda e: e.tensor_tensor(hf[:, :], hf[:, :], hb[:, :], ALU.add), reads=[tk["hf"], tk["hb"]], writes=[tk["hf"]])
        ph.dve(lambda e: e.tensor_tensor(ob[:, :], hf[:, :], r[:, :], ALU.mult), reads=[tk["hf"], tk["r"]], writes=[tob])
        ph.store(dr["MIXT"][n * 128:(n + 1) * 128, :], ob[:, :], reads=[tob])
    ph.finish()


Prog.phase_p2 = phase_p2


CH = 16


def phase_p3(self, l):
    K, dr, par = self.K, self.dr, self.par
    ph = Phase(K, "p3_%d" % l)
    identB, onesF, tri, rmask, low, oml = self.identB, self.onesF, self.tri, self.rmask, self.low, self.oml
    ogn, _ = self.pv("gnorm", l)
    of_ = ph.sb("of", [128, T], F32)
    tof = Tok()
    Vh = ph.sb("Vh", [CH, T // CH, 128], BF16)
    tV = Tok()
    S = ph.sb("S", [128, 128], F32)
    Sb = [ph.sb("Sb", [128, 128], BF16) for _ in range(2)]
    tS = Tok()
    tSb = [Tok() for _ in range(2)]
    sp = 0
    nm = ["qr", "fl", "q", "f", "g", "k", "bc", "e1", "e2", "og", "sq", "rs", "t1"]
    bl = {n: ph.sb(n, [128, 512], F32) for n in nm}
    tb = {n: Tok() for n in nm}
    Qt = ph.sb("Qt", [128, 512], BF16)
    Kt = ph.sb("Kt", [128, 512], BF16)
    Kh = ph.sb("Kh", [128, 512], BF16)
    tQt, tKt, tKh = Tok(), Tok(), Tok()
    yb = ph.sb("yb", [128, 512], BF16)
    tyb = Tok()
    ATm = [ph.sb("ATm", [CH, CH], BF16) for _ in range(2)]
    tATm = [Tok() for _ in range(2)]
    KhT = [ph.sb("KhT", [CH, 128], BF16) for _ in range(2)]
    tKhT = [Tok() for _ in range(2)]
    psA = [ph.ps("psA", [CH, CH]) for _ in range(2)]
    tpsA = [Tok() for _ in range(2)]
    psT = [ph.ps("psT", [CH, 128], BF16) for _ in range(2)]
    tpsT = [Tok() for _ in range(2)]
    psKV = [ph.ps("psKV", [128, 128]) for _ in range(2)]
    tpsKV = [Tok() for _ in range(2)]
    psO = ph.ps("psO", [128, 512])
    tpsO = Tok()
    psM = ph.ps("psM", [128, 512])
    tpsM = Tok()
    cc = 0
    for h in range(4):
        ph.load(Vh[:, :, :], dr["UBI"][:, h * 128:(h + 1) * 128].rearrange("(c s) v -> s c v", s=CH), writes=[tV])
        for d in range(2):
            lowv = low[:, d, l, h:h + 1]
            omlv = oml[:, d, l, h:h + 1]
            ph.dve(lambda e: e.memset(S[:, :], 0.0), writes=[tS])
            ph.dve(lambda e, sp=sp: e.memset(Sb[sp][:, :], 0.0), writes=[tSb[sp]])
            order = list(range(9)) if d == 0 else [0] + list(range(8, 0, -1))
            for bi in order:
                b0, w = BLOCKS[bi]
                nch = w // CH
                ph.load(bl["qr"][:, 0:w], dr["UBT"][h * 128:(h + 1) * 128, b0:b0 + w], writes=[tb["qr"]])
                ph.load(bl["fl"][:, 0:w], dr["UBT"][512 + d * 512 + h * 128:512 + d * 512 + (h + 1) * 128, b0:b0 + w], writes=[tb["fl"]])
                ph.act(lambda e, w=w: e.activation(out=bl["q"][:, 0:w], in_=bl["qr"][:, 0:w], func=AF.Silu), reads=[tb["qr"]], writes=[tb["q"]])
                ph.act(lambda e, w=w: e.activation(out=bl["f"][:, 0:w], in_=bl["fl"][:, 0:w], func=AF.Sigmoid), reads=[tb["fl"]], writes=[tb["f"]])
                ph.dve(lambda e, w=w, lowv=lowv, omlv=omlv: e.tensor_scalar(bl["f"][:, 0:w], bl["f"][:, 0:w], omlv, lowv, ALU.mult, ALU.add),
                       reads=[tb["f"]], writes=[tb["f"]])
                ph.act(lambda e, w=w: e.activation(out=bl["g"][:, 0:w], in_=bl["f"][:, 0:w], func=AF.Ln), reads=[tb["f"]], writes=[tb["g"]])
                ph.dve(lambda e, w=w: e.tensor_scalar(bl["k"][:, 0:w], bl["f"][:, 0:w], -1.0, 1.0, ALU.mult, ALU.add), reads=[tb["f"]], writes=[tb["k"]])
                if d == 0:
                    ph.dve(lambda e, w=w: e.tensor_tensor_scan(bl["bc"][:, 0:w], rmask[:, 0:w], bl["g"][:, 0:w], 0.0, ALU.mult, ALU.add),
                           reads=[tb["g"]], writes=[tb["bc"]])
                else:
                    ph.dve(lambda e, w=w: e.tensor_tensor_scan(rev(bl["bc"][:, 0:w]), rmask[:, 0:w], rev(bl["g"][:, 0:w]), 0.0, ALU.mult, ALU.add),
                           reads=[tb["g"]], writes=[tb["bc"]])
                ph.act(lambda e, w=w: e.activation(out=bl["e1"][:, 0:w], in_=bl["bc"][:, 0:w], func=AF.Exp), reads=[tb["bc"]], writes=[tb["e1"]])
                ph.act(lambda e, w=w: e.activation(out=bl["e2"][:, 0:w], in_=bl["bc"][:, 0:w], func=AF.Exp, scale=-1.0), reads=[tb["bc"]], writes=[tb["e2"]])
                ph.dve(lambda e, w=w: e.tensor_tensor(Qt[:, 0:w], bl["q"][:, 0:w], bl["e1"][:, 0:w], ALU.mult), reads=[tb["q"], tb["e1"]], writes=[tQt])
                ph.dve(lambda e, w=w: e.tensor_tensor(Kt[:, 0:w], bl["k"][:, 0:w], bl["e2"][:, 0:w], ALU.mult), reads=[tb["k"], tb["e2"]], writes=[tKt])
                eo = (CH - 1) if d == 0 else 0
                decv = bl["e1"][:, eo:w:CH]
                ph.dve(lambda e, w=w, nch=nch, decv=decv: e.tensor_tensor(
                    Kh[:, 0:w].rearrange("p (c s) -> p c s", s=CH), Kt[:, 0:w].rearrange("p (c s) -> p c s", s=CH),
                    bc_last(decv, CH), ALU.mult), reads=[tKt, tb["e1"]], writes=[tKh])
                chs = list(range(nch)) if d == 0 else list(range(nch - 1, -1, -1))
                cbase = cc
                cc += nch

                def stepA(i, chs=chs, cbase=cbase, b0=b0, d=d):
                    o = chs[i] * CH
                    gc = (b0 + o) // CH
                    p = (cbase + i) % 2
                    ph.pe(lambda e, p=p, o=o: e.matmul(psA[p][:, :], Kt[:, o:o + CH], Qt[:, o:o + CH], start=True, stop=True),
                          reads=[tKt, tQt], writes=[tpsA[p]])
                    ph.dve(lambda e, p=p, d=d: e.tensor_tensor(ATm[p][:, :], psA[p][:, :], tri[0:CH, d, 0:CH], ALU.mult),
                           reads=[tpsA[p]], writes=[tATm[p]])
                    ph.pe(lambda e, p=p, o=o: e.transpose(psT[p][:, :], Kh[:, o:o + CH], identB[:, :]), reads=[tKh], writes=[tpsT[p]])
                    ph.act(lambda e, p=p: e.copy(KhT[p][:, :], psT[p][:, :]), reads=[tpsT[p]], writes=[tKhT[p]])

                def stepA2(i, chs=chs, cbase=cbase, b0=b0):
                    o = chs[i] * CH
                    gc = (b0 + o) // CH
                    p = (cbase + i) % 2
                    ph.pe(lambda e, p=p, gc=gc: e.matmul(psKV[p][:, :], KhT[p][:, :], Vh[:, gc, :], start=True, stop=True),
                          reads=[tKhT[p], tV], writes=[tpsKV[p]])

                def stepO(i, chs=chs, cbase=cbase, b0=b0):
                    o = chs[i] * CH
                    gc = (b0 + o) // CH
                    p = (cbase + i) % 2
                    s0 = sp
                    ph.pe(lambda e, o=o, s0=s0: e.matmul(psO[:, o:o + CH], Sb[s0][:, :], Qt[:, o:o + CH], start=True, stop=False),
                          reads=[tSb[s0], tQt], writes=[tpsO])
                    ph.pe(lambda e, o=o, p=p, gc=gc: e.matmul(psO[:, o:o + CH], Vh[:, gc, :], ATm[p][:, :], start=False, stop=True),
                          reads=[tV, tATm[p]], writes=[tpsO])

                def stepU(i, chs=chs, cbase=cbase, b0=b0, eo=eo):
                    nonlocal sp
                    o = chs[i] * CH
                    p = (cbase + i) % 2
                    s1 = 1 - sp
                    sp = s1
                    dsc = bl["e1"][:, o + eo:o + eo + 1]
                    ph.dve(lambda e, p=p, dsc=dsc: e.scalar_tensor_tensor(S[:, :], S[:, :], dsc, psKV[p][:, :], ALU.mult, ALU.add),
                           reads=[tS, tpsKV[p], tb["e1"]], writes=[tS])
                    ph.act(lambda e, s1=s1: e.copy(Sb[s1][:, :], S[:, :]), reads=[tS], writes=[tSb[s1]])

                for i in range(nch + 1):
                    if i < nch:
                        stepA(i)
                    if i >= 1:
                        stepO(i - 1)
                    if i < nch:
                        stepA2(i)
                    if i >= 1:
                        stepU(i - 1)
                if d == 0:
                    ph.act(lambda e, b0=b0, w=w: e.copy(of_[:, b0:b0 + w], psO[:, 0:w]), reads=[tpsO], writes=[tof])
                else:
                    ph.dve(lambda e, b0=b0, w=w: e.tensor_tensor(of_[:, b0:b0 + w], of_[:, b0:b0 + w], psO[:, 0:w], ALU.add),
                           reads=[tpsO, tof], writes=[tof])
                    ph.act(lambda e, b0=b0, w=w: e.activation(out=bl["sq"][:, 0:w], in_=of_[:, b0:b0 + w], func=AF.Square), reads=[tof], writes=[tb["sq"]])
                    ph.pe(lambda e, w=w: e.matmul(psM[:, 0:w], onesF[:, :], bl["sq"][:, 0:w], start=True, stop=True), reads=[tb["sq"]], writes=[tpsM])
                    ph.act(lambda e, w=w: e.activation(out=bl["rs"][:, 0:w], in_=psM[:, 0:w], func=AF.Sqrt, bias=self.epsb[:, :]), reads=[tpsM], writes=[tb["rs"]])
                    ph.dve(lambda e, w=w: e.reciprocal(bl["rs"][:, 0:w], bl["rs"][:, 0:w]), reads=[tb["rs"]], writes=[tb["rs"]])
                    ph.load(bl["og"][:, 0:w], dr["UBT"][1536 + h * 128:1536 + (h + 1) * 128, b0:b0 + w], writes=[tb["og"]])
                    ph.act(lambda e, w=w: e.activation(out=bl["og"][:, 0:w], in_=bl["og"][:, 0:w], func=AF.Silu), reads=[tb["og"]], writes=[tb["og"]])
                    ph.dve(lambda e, b0=b0, w=w: e.tensor_tensor(bl["t1"][:, 0:w], of_[:, b0:b0 + w], bl["rs"][:, 0:w], ALU.mult),
                           reads=[tof, tb["rs"]], writes=[tb["t1"]])
                    ph.dve(lambda e, w=w: e.scalar_tensor_tensor(yb[:, 0:w], bl["t1"][:, 0:w], par[:, ogn:ogn + 1], bl["og"][:, 0:w], ALU.mult, ALU.mult),
                           reads=[tb["t1"], tb["og"]], writes=[tyb])
                    ph.store(dr["MIXT"][512 + h * 128:512 + (h + 1) * 128, b0:b0 + w], yb[:, 0:w], reads=[tyb])
    if "DBG" in self.debug:
        ph.store(dr["DBG"][0, :, :], of_[:, :], reads=[tof])
        for i_, n_ in enumerate(["f", "g", "k", "bc", "e1", "e2", "rs", "t1", "og", "q"]):
            ph.store(dr["DBG"][1 + i_, :, 0:512], bl[n_][:, :], reads=[tb[n_]])
    ph.finish()


Prog.phase_p3 = phase_p3


def phase_p4a(self, l):
    K, dr = self.K, self.dr
    ph = Phase(K, "p4a_%d" % l)
    identB = self.identB
    gain = ph.sb("gain", [128, 10, 128], F32)
    tg = Tok()
    for hd in range(10):
        src = dr["q_norm"][l, :] if hd < 8 else dr["k_norm"][l, :]
        ph.load(gain[:, hd, :], src.partition_broadcast(128), writes=[tg])
    uc = [ph.sb("uc", [128, 1536], F32) for _ in range(2)]
    tuc = [Tok() for _ in range(2)]
    cs = [ph.sb("cs", [128, 2, 2, 2, 32], F32) for _ in range(2)]
    tcs = [Tok() for _ in range(2)]
    sq = ph.sb("sq", [128, 10, 128], F32)
    tsq = Tok()
    ss = [ph.sb("ss", [128, 10], F32) for _ in range(2)]
    tss = [Tok() for _ in range(2)]
    xn = ph.sb("xn", [128, 10, 2, 2, 32], F32)
    txn = Tok()
    t1 = ph.sb("t1", [128, 10, 2, 2, 32], F32)
    tt1 = Tok()
    t2 = ph.sb("t2", [128, 10, 2, 2, 32], F32)
    tt2 = Tok()
    qkb = [ph.sb("qkb", [128, 10, 128], BF16) for _ in range(2)]
    tqkb = [Tok() for _ in range(2)]
    vb = [ph.sb("vb", [128, 256], BF16) for _ in range(2)]
    tvb = [Tok() for _ in range(2)]
    psQ = [ph.ps("psQ", [128, 8, 128], BF16) for _ in range(2)]
    tpsQ = [Tok() for _ in range(2)]
    psK = [ph.ps("psK", [128, 2, 128], BF16) for _ in range(2)]
    tpsK = [Tok() for _ in range(2)]
    qst = [ph.sb("qst", [128, 10, 512], BF16) for _ in range(2)]
    tqst = [Tok() for _ in range(2)]
    it = 0
    for bi, (b0, w) in enumerate(BLOCKS):
        sb_ = bi % 2
        for tt in range(w // 128):
            t0 = b0 + tt * 128
            p = it % 2
            it += 1
            ph.load(uc[p][:, :], dr["UC"][t0:t0 + 128, :], writes=[tuc[p]])
            ph.load(cs[p][:, :, :, :, :], dr["rope"][t0:t0 + 128, :].rearrange("t (a b h f) -> t a b h f", a=2, b=2, h=2), writes=[tcs[p]])
            ucv = uc[p][:, 0:1280].rearrange("t (h d) -> t h d", d=128)
            ph.act(lambda e, ucv=ucv: e.activation(out=sq[:, :, :], in_=ucv, func=AF.Square), reads=[tuc[p]], writes=[tsq])
            ph.dve(lambda e, p=p: e.tensor_reduce(ss[p][:, :], sq[:, :, :], AX.X, ALU.add), reads=[tsq], writes=[tss[p]])
            ph.act(lambda e, p=p: e.activation(out=ss[p][:, :], in_=ss[p][:, :], func=AF.Sqrt, scale=1.0 / 128.0, bias=self.epsb[:, :]),
                   reads=[tss[p]], writes=[tss[p]])
            ph.dve(lambda e, p=p: e.reciprocal(ss[p][:, :], ss[p][:, :]), reads=[tss[p]], writes=[tss[p]])
            xnv = xn[:, :, :, :, :].rearrange("t h b x f -> t h (b x f)")
            ph.dve(lambda e, p=p, ucv=ucv, xnv=xnv: e.tensor_tensor(xnv, ucv, bc_last(ss[p][:, :], 128), ALU.mult),
                   reads=[tuc[p], tss[p]], writes=[txn])
            ph.dve(lambda e, xnv=xnv: e.tensor_tensor(xnv, xnv, gain[:, :, :], ALU.mult), reads=[txn, tg], writes=[txn])
            cosv = cs[p][:, 0, :, :, :].rearrange("t b x f -> t (b x f)")
            t1v = t1[:, :, :, :, :].rearrange("t h b x f -> t h (b x f)")
            ph.dve(lambda e, xnv=xnv, t1v=t1v, cosv=cosv: e.tensor_tensor(t1v, xnv, bc_mid(cosv, 10), ALU.mult),
                   reads=[txn, tcs[p]], writes=[tt1])
            for hf in range(2):
                sinv = cs[p][:, 1, :, hf, :]
                ph.dve(lambda e, hf=hf, sinv=sinv: e.tensor_tensor(t2[:, :, :, hf, :], xn[:, :, :, 1 - hf, :], bc_mid(sinv, 10), ALU.mult),
                       reads=[txn, tcs[p]], writes=[tt2])
            t2v = t2[:, :, :, :, :].rearrange("t h b x f -> t h (b x f)")
            ph.dve(lambda e, p=p, t1v=t1v, t2v=t2v: e.tensor_tensor(qkb[p][:, :, :], t1v, t2v, ALU.add), reads=[tt1, tt2], writes=[tqkb[p]])
            for hd in range(10):
                if hd < 8:
                    ph.pe(lambda e, p=p, hd=hd: e.transpose(psQ[p][:, hd, :], qkb[p][:, hd, :], identB[:, :]), reads=[tqkb[p]], writes=[tpsQ[p]])
                else:
                    ph.pe(lambda e, p=p, hd=hd: e.transpose(psK[p][:, hd - 8, :], qkb[p][:, hd, :], identB[:, :]), reads=[tqkb[p]], writes=[tpsK[p]])
            c0 = tt * 128
            ph.act(lambda e, p=p, sb_=sb_, c0=c0: e.copy(qst[sb_][:, 0:8, c0:c0 + 128], psQ[p][:, :, :]), reads=[tpsQ[p]], writes=[tqst[sb_]])
            ph.act(lambda e, p=p, sb_=sb_, c0=c0: e.copy(qst[sb_][:, 8:10, c0:c0 + 128], psK[p][:, :, :]), reads=[tpsK[p]], writes=[tqst[sb_]])
            ph.pool(lambda e, p=p: e.tensor_copy(vb[p][:, :], uc[p][:, 1280:1536]), reads=[tuc[p]], writes=[tvb[p]])
            ph.store(dr["V"][t0:t0 + 128, :], vb[p][:, :], reads=[tvb[p]])
        ph.store(dr["QT"][:, :, b0:b0 + w].rearrange("h d t -> d h t"), qst[sb_][:, 0:8, 0:w], reads=[tqst[sb_]])
        ph.store(dr["KT"][:, :, b0:b0 + w].rearrange("h d t -> d h t"), qst[sb_][:, 8:10, 0:w], reads=[tqst[sb_]])
    ph.finish()


def phase_p4b(self, l):
    K, dr = self.K, self.dr
    ph = Phase(K, "p4b_%d" % l)
    onesB = self.onesB
    KTn = ph.sb("KTn", [128, T], BF16)
    Vn = ph.sb("Vn", [128, NT, 128], BF16)
    tKT, tV = Tok(), Tok()
    Qb = [ph.sb("Qb", [128, 512], BF16) for _ in range(2)]
    tQb = [Tok() for _ in range(2)]
    Pt = [ph.sb("Pt", [128, 512], BF16) for _ in range(3)]
    tPt = [Tok() for _ in range(3)]
    psS = [ph.ps("psS", [128, 512]) for _ in range(2)]
    tpsS = [Tok() for _ in range(2)]
    psO = [ph.ps("psO", [128, 512]) for _ in range(2)]
    tpsO = [Tok() for _ in range(2)]
    psR = [ph.ps("psR", [128, 512]) for _ in range(2)]
    tpsR = [Tok() for _ in range(2)]
    rinv = [ph.sb("rinv", [128, 512], F32) for _ in range(2)]
    trinv = [Tok() for _ in range(2)]
    ob = [ph.sb("ob", [128, 512], BF16) for _ in range(2)]
    tob = [Tok() for _ in range(2)]
    nq = 0
    ns = 0
    npt = 0
    for n in range(2):
        ph.load(KTn[:, :], dr["KT"][n, :, :], writes=[tKT])
        ph.load(Vn[:, :, :], dr["V"][:, n * 128:(n + 1) * 128].rearrange("(j p) c -> p j c", p=128), writes=[tV])
        for g in range(4):
            hd = n * 4 + g
            for bi, (b0, w) in enumerate(BLOCKS):
                kts = [0, 1] if bi == 0 else list(range(NT))
                qi = nq % 2
                nq += 1
                ph.load(Qb[qi][:, 0:w], dr["QT"][hd, :, b0:b0 + w], writes=[tQb[qi]])

                def smm(kt, qi=qi, w=w):
                    nonlocal ns
                    si = ns % 2
                    ns += 1
                    ph.pe(lambda e, si=si, kt=kt, qi=qi, w=w: e.matmul(psS[si][:, 0:w], KTn[:, kt * 128:(kt + 1) * 128], Qb[qi][:, 0:w], start=True, stop=True),
                          reads=[tKT, tQb[qi]], writes=[tpsS[si]])
                    return si
                si = smm(kts[0])
                for j, kt in enumerate(kts):
                    si_next = smm(kts[j + 1]) if j + 1 < len(kts) else None
                    pi = npt % 3
                    npt += 1
                    ph.act(lambda e, si=si, pi=pi, w=w: e.activation(out=Pt[pi][:, 0:w], in_=psS[si][:, 0:w], func=AF.Exp, scale=ATT_SCALE),
                           reads=[tpsS[si]], writes=[tPt[pi]])
                    ph.pe(lambda e, qi=qi, kt=kt, pi=pi, w=w, j=j, nk=len(kts): e.matmul(psO[qi][:, 0:w], Vn[:, kt, :], Pt[pi][:, 0:w], start=(j == 0), stop=(j == nk - 1)),
                          reads=[tV, tPt[pi]], writes=[tpsO[qi]])
                    ph.pe(lambda e, qi=qi, pi=pi, w=w, j=j, nk=len(kts): e.matmul(psR[qi][:, 0:w], onesB[:, :], Pt[pi][:, 0:w], start=(j == 0), stop=(j == nk - 1)),
                          reads=[tPt[pi]], writes=[tpsR[qi]])
                    si = si_next
                ph.dve(lambda e, qi=qi, w=w: e.reciprocal(rinv[qi][:, 0:w], psR[qi][:, 0:w]), reads=[tpsR[qi]], writes=[trinv[qi]])
                ph.dve(lambda e, qi=qi, w=w: e.tensor_tensor(ob[qi][:, 0:w], psO[qi][:, 0:w], rinv[qi][:, 0:w], ALU.mult),
                       reads=[tpsO[qi], trinv[qi]], writes=[tob[qi]])
                ph.store(dr["MIXT"][1024 + hd * 128:1024 + (hd + 1) * 128, b0:b0 + w], ob[qi][:, 0:w], reads=[tob[qi]])
    ph.finish()


Prog.phase_p4a = phase_p4a
Prog.phase_p4b = phase_p4b


def load_cast_weight(ph, dst_bf, tdst, src_ap_fn, nchunks, stage, tstage, cast_engs, ctr):
    for c in range(nchunks):
        j = ctr[0] % len(stage)
        ctr[0] += 1
        src, dstv = src_ap_fn(c)
        ph.load(stage[j], src, writes=[tstage[j]]) if False else None
        yield c, j, src, dstv


def phase_p5(self, l):
    K, dr = self.K, self.dr
    ph = Phase(K, "p5_%d" % l)
    ll = l if self.n_layers > 1 else 0
    wo = ph.sb("wo", [128, KC, D], BF16)
    two = Tok()
    stg = [ph.sb("stg", [128, KC, 256], F32) for _ in range(2)]
    tstg = [Tok() for _ in range(2)]
    wsrc = dr["w_out"][ll].rearrange("(k p) n -> p k n", p=128)
    for c in range(D // 256):
        j = c % 2
        ph.load(stg[j][:, :, :], wsrc[:, :, c * 256:(c + 1) * 256], writes=[tstg[j]])
        ph.pool(lambda e, j=j, c=c: e.tensor_copy(wo[:, :, c * 256:(c + 1) * 256], stg[j][:, :, :]), reads=[tstg[j]], writes=[two])
    gbc = [ph.sb("gbc", [128, D], F32) for _ in range(2)]
    tg = Tok()
    for ty in range(2):
        ph.load(gbc[ty][:, :], dr["MODROW"][l, ty, 2 * D:3 * D].partition_broadcast(128), writes=[tg])
    mx = [ph.sb("mx", [128, KC, 512], BF16) for _ in range(2)]
    tmx = [Tok() for _ in range(2)]
    xt = [ph.sb("xt", [128, D], F32) for _ in range(2)]
    txt = [Tok() for _ in range(2)]
    tmp = [ph.sb("tmp", [128, 512], F32) for _ in range(2)]
    ttmp = [Tok() for _ in range(2)]
    pm = [ph.ps("pm", [128, 512]) for _ in range(4)]
    tpm = [Tok() for _ in range(4)]
    msrc = dr["MIXT"].rearrange("(k p) t -> p k t", p=128)
    it = 0
    nm = 0
    for bi, (b0, w) in enumerate(BLOCKS):
        mi = bi % 2
        ph.load(mx[mi][:, :, 0:w], msrc[:, :, b0:b0 + w], writes=[tmx[mi]])
        for tt in range(w // 128):
            t0 = b0 + tt * 128
            ty = 1 if t0 < NCTX else 0
            p = it % 2
            it += 1
            ph.load(xt[p][:, :], dr["X"][t0:t0 + 128, :], writes=[txt[p]])
            for dc in range(4):
                q = nm % 4
                nm += 1
                for k in range(KC):
                    ph.pe(lambda e, q=q, mi=mi, k=k, tt=tt, dc=dc: e.matmul(pm[q][:, :], mx[mi][:, k, tt * 128:(tt + 1) * 128], wo[:, k, dc * 512:(dc + 1) * 512],
                                                                             start=(k == 0), stop=(k == KC - 1)),
                          reads=[tmx[mi], two], writes=[tpm[q]])
                tp = nm % 2
                ph.dve(lambda e, q=q, tp=tp, ty=ty, dc=dc: e.tensor_tensor(tmp[tp][:, :], pm[q][:, :], gbc[ty][:, dc * 512:(dc + 1) * 512], ALU.mult),
                       reads=[tpm[q], tg], writes=[ttmp[tp]])
                ph.pool(lambda e, p=p, tp=tp, dc=dc: e.tensor_tensor(xt[p][:, dc * 512:(dc + 1) * 512], xt[p][:, dc * 512:(dc + 1) * 512], tmp[tp][:, :], ALU.add),
                        reads=[ttmp[tp], txt[p]], writes=[txt[p]])
            ph.store(dr["X"][t0:t0 + 128, :], xt[p][:, :], reads=[txt[p]])
    ph.finish()


def phase_p6a(self, l):
    K, dr = self.K, self.dr
    ph = Phase(K, "p6a_%d" % l)
    moe = (l % 2 == 1)
    identF = self.identF
    hst = ph.sb("hst", [128, KC, 512], BF16)
    thst = Tok()
    if moe:
        rt = ph.sb("rt", [128, KC, NE], F32)
        trt = Tok()
        ph.load(rt[:, :, :], dr["router"][l // 2].rearrange("(k p) e -> p k e", p=128), writes=[trt])
        h2f = ph.sb("h2f", [128, KC, 128], F32)
        th2f = Tok()
        psL = ph.ps("psL", [128, NE])
        tpsL = Tok()
        psG = ph.ps("psG", [NE, 128])
        tpsG = Tok()
        sm = {n: ph.sb(n, [128, NE], F32) for n in ("lg", "eq1", "lg2", "eq2", "gt")}
        s1 = {n: ph.sb(n, [128, 1], F32) for n in ("m1", "m2", "dm", "w1", "w2")}
        tsm = Tok()
        gst = ph.sb("gst", [NE, 512], F32)
        tgst = Tok()

    for bi, (b0, w) in enumerate(BLOCKS):
        def extra(t0, col, ty, st, b0=b0):
            if not moe:
                return
            der = self.der
            for k in range(KC):
                gsv = der[:, l, ty, 2, k:k + 1]
                shv = der[:, l, ty, 3, k:k + 1]
                src = st["pT"][k // 4][:, k % 4, :]
                ph.pool_or = None
                ph.dve(lambda e, k=k, src=src, gsv=gsv, shv=shv: e.tensor_scalar(h2f[:, k, :], src, gsv, shv, ALU.mult, ALU.add),
                       reads=[st["tpT"][k // 4]], writes=[th2f])
            for k in range(KC):
                ph.pe(lambda e, k=k: e.matmul(psL[:, :], h2f[:, k, :], rt[:, k, :], start=(k == 0), stop=(k == KC - 1)),
                      reads=[th2f, trt], writes=[tpsL])
            lg, eq1, lg2, eq2, gt = sm["lg"], sm["eq1"], sm["lg2"], sm["eq2"], sm["gt"]
            m1, m2, dm, w1, w2 = s1["m1"], s1["m2"], s1["dm"], s1["w1"], s1["w2"]
            R, W = [tsm, tpsL], [tsm]
            ph.act(lambda e: e.copy(lg[:, :], psL[:, :]), reads=R, writes=W)
            ph.dve(lambda e: e.tensor_reduce(m1[:, :], lg[:, :], AX.X, ALU.max), reads=[tsm], writes=W)
            ph.dve(lambda e: e.tensor_scalar(eq1[:, :], lg[:, :], m1[:, :], None, ALU.is_equal), reads=[tsm], writes=W)
            ph.dve(lambda e: e.scalar_tensor_tensor(lg2[:, :], eq1[:, :], -1e30, lg[:, :], ALU.mult, ALU.add), reads=[tsm], writes=W)
            ph.dve(lambda e: e.tensor_reduce(m2[:, :], lg2[:, :], AX.X, ALU.max), reads=[tsm], writes=W)
            ph.dve(lambda e: e.tensor_scalar(eq2[:, :], lg2[:, :], m2[:, :], None, ALU.is_equal), reads=[tsm], writes=W)
            ph.dve(lambda e: e.tensor_tensor(dm[:, :], m2[:, :], m1[:, :], ALU.subtract), reads=[tsm], writes=W)
            ph.act(lambda e: e.activation(out=dm[:, :], in_=dm[:, :], func=AF.Exp), reads=[tsm], writes=W)
            ph.dve(lambda e: e.tensor_scalar(w1[:, :], dm[:, :], 1.0, None, ALU.add), reads=[tsm], writes=W)
            ph.dve(lambda e: e.reciprocal(w1[:, :], w1[:, :]), reads=[tsm], writes=W)
            ph.dve(lambda e: e.tensor_tensor(w2[:, :], dm[:, :], w1[:, :], ALU.mult), reads=[tsm], writes=W)
            ph.dve(lambda e: e.tensor_scalar(gt[:, :], eq1[:, :], w1[:, :], None, ALU.mult), reads=[tsm], writes=W)
            ph.dve(lambda e: e.scalar_tensor_tensor(gt[:, :], eq2[:, :], w2[:, :], gt[:, :], ALU.mult, ALU.add), reads=[tsm], writes=W)
            ph.pe(lambda e: e.transpose(psG[:, :], gt[:, :], identF[:, :]), reads=[tsm], writes=[tpsG])
            c0 = t0 - b0
            ph.act(lambda e, c0=c0: e.copy(gst[:, c0:c0 + 128], psG[:, :]), reads=[tpsG], writes=[tgst])

        norm_tiles(self, ph, l, 1, [(b0, w)], hst, thst, f32_out=extra)
        ph.store(dr["H2T"][:, :, b0:b0 + w], hst[:, :, 0:w], reads=[thst])
        if moe:
            ph.store(dr["GT"][:, b0:b0 + w], gst[:, 0:w], reads=[tgst])
    ph.finish()


Prog.phase_p5 = phase_p5
Prog.phase_p6a = phase_p6a


def phase_p6b(self, l):
    K, dr = self.K, self.dr
    moe = (l % 2 == 1)
    jj = l // 2
    if moe:
        ne = NE if self.n_layers > 1 else 1
        w1s = [dr["moe_w1"][jj, e] for e in range(ne)]
        w3s = [dr["moe_w3"][jj, e] for e in range(ne)]
        w2s = [dr["moe_w2"][jj, e] for e in range(ne)]
    else:
        ne = 1
        w1s, w3s, w2s = [dr["ffn_w1"][jj]], [dr["ffn_w3"][jj]], [dr["ffn_w2"][jj]]
    sel = self.sel

    ph = Phase(K, "p6b1_%d" % l)
    h2g = ph.sb("h2g", [128, KC, TGMAX], BF16)
    th2g = Tok()
    FW = 256
    st1 = ph.sb("st1", [128, KC, FW], F32)
    st3 = ph.sb("st3", [128, KC, FW], F32)
    tst1, tst3 = Tok(), Tok()
    w1b = [ph.sb("w1b", [128, KC, FW], BF16) for _ in range(2)]
    w3b = [ph.sb("w3b", [128, KC, FW], BF16) for _ in range(2)]
    tw1b = [Tok() for _ in range(2)]
    tw3b = [Tok() for _ in range(2)]
    psG = [ph.ps("psG", [128, 512]) for _ in range(2)]
    psU = [ph.ps("psU", [128, 512]) for _ in range(2)]
    tpsG = [Tok() for _ in range(2)]
    tpsU = [Tok() for _ in range(2)]
    sg = [ph.sb("sg", [128, 512], F32) for _ in range(2)]
    tsg = [Tok() for _ in range(2)]
    ast = [ph.sb("ast", [128, 512], BF16) for _ in range(3)]
    tast = [Tok() for _ in range(3)]
    if moe:
        gT = ph.sb("gT", [NE, TGMAX], F32)
        tgT = Tok()
        psB = [ph.ps("psB", [128, 512]) for _ in range(2)]
        tpsB = [Tok() for _ in range(2)]
        tmp = [ph.sb("tmpa", [128, 512], F32) for _ in range(2)]
        ttmp = [Tok() for _ in range(2)]
    nw = 0
    nb = 0
    na = 0
    for grp in GROUPS:
        g0 = grp[0][0]
        gw = sum(w for _, w in grp)
        ph.load(h2g[:, :, 0:gw], dr["H2T"][:, :, g0:g0 + gw], writes=[th2g])
        if moe:
            ph.load(gT[:, 0:gw], dr["GT"][:, g0:g0 + gw], writes=[tgT])
        for ex in range(ne):
            w1src = w1s[ex].rearrange("(k p) f -> p k f", p=128)
            w3src = w3s[ex].rearrange("(k p) f -> p k f", p=128)
            for fp in range(DFF // FW):
                j = nw % 2
                nw += 1
                ph.load(st1[:, :, :], w1src[:, :, fp * FW:(fp + 1) * FW], writes=[tst1])
                ph.load(st3[:, :, :], w3src[:, :, fp * FW:(fp + 1) * FW], writes=[tst3])
                ph.pool(lambda e, j=j: e.tensor_copy(w1b[j][:, :, :], st1[:, :, :]), reads=[tst1], writes=[tw1b[j]])
                ph.pool(lambda e, j=j: e.tensor_copy(w3b[j][:, :, :], st3[:, :, :]), reads=[tst3], writes=[tw3b[j]])
                for (b0, w) in grp:
                    bidx = BLOCKS.index((b0, w))
                    col = b0 - g0
                    for m in range(FW // 128):
                        fc = fp * (FW // 128) + m
                        q = nb % 2
                        nb += 1
                        for k in range(KC):
                            ph.pe(lambda e, q=q, j=j, k=k, m=m, col=col, w=w: e.matmul(psG[q][:, 0:w], w1b[j][:, k, m * 128:(m + 1) * 128], h2g[:, k, col:col + w],
                                                                                       start=(k == 0), stop=(k == KC - 1)),
                                  reads=[tw1b[j], th2g], writes=[tpsG[q]])
                        for k in range(KC):
                            ph.pe(lambda e, q=q, j=j, k=k, m=m, col=col, w=w: e.matmul(psU[q][:, 0:w], w3b[j][:, k, m * 128:(m + 1) * 128], h2g[:, k, col:col + w],
                                                                                       start=(k == 0), stop=(k == KC - 1)),
                                  reads=[tw3b[j], th2g], writes=[tpsU[q]])
                        ph.act(lambda e, q=q, w=w: e.activation(out=sg[q][:, 0:w], in_=psG[q][:, 0:w], func=AF.Silu), reads=[tpsG[q]], writes=[tsg[q]])
                        ai = na % 3
                        na += 1
                        if not moe:
                            ph.dve(lambda e, q=q, ai=ai, w=w: e.tensor_tensor(ast[ai][:, 0:w], psU[q][:, 0:w], sg[q][:, 0:w], ALU.mult),
                                   reads=[tpsU[q], tsg[q]], writes=[tast[ai]])
                        else:
                            ph.pe(lambda e, q=q, ex=ex, col=col, w=w: e.matmul(psB[q][:, 0:w], sel[:, ex, :], gT[:, col:col + w], start=True, stop=True),
                                  reads=[tgT], writes=[tpsB[q]])
                            ph.dve(lambda e, q=q, w=w: e.tensor_tensor(tmp[q][:, 0:w], psU[q][:, 0:w], sg[q][:, 0:w], ALU.mult),
                                   reads=[tpsU[q], tsg[q]], writes=[ttmp[q]])
                            ph.dve(lambda e, q=q, ai=ai, w=w: e.tensor_tensor(ast[ai][:, 0:w], psB[q][:, 0:w], tmp[q][:, 0:w], ALU.mult),
                                   reads=[tpsB[q], ttmp[q]], writes=[tast[ai]])
                        ph.store(dr["AT%d" % ex][bidx, :, fc, 0:w], ast[ai][:, 0:w], reads=[tast[ai]])
    ph.finish()

    ph = Phase(K, "p6b2_%d" % l)
    w2b = ph.sb("w2b", [128, FC, 512], BF16)
    tw2b = Tok()
    stg = [ph.sb("stg2", [128, 4, 512], F32) for _ in range(2)]
    tstg = [Tok() for _ in range(2)]
    aT = [ph.sb("aT", [128, FC, 512], BF16) for _ in range(2)]
    taT = [Tok() for _ in range(2)]
    gbc = [ph.sb("g5", [128, D], F32) for _ in range(2)]
    tg = Tok()
    for ty in range(2):
        ph.load(gbc[ty][:, :], dr["MODROW"][l, ty, 5 * D:6 * D].partition_broadcast(128), writes=[tg])
    xq = [ph.sb("xq", [128, 512], F32) for _ in range(3)]
    txq = [Tok() for _ in range(3)]
    tmp2 = [ph.sb("tmp2", [128, 512], F32) for _ in range(2)]
    ttmp2 = [Tok() for _ in range(2)]
    pm = [ph.ps("pm2", [128, 512]) for _ in range(4)]
    tpm = [Tok() for _ in range(4)]
    xtok = {}
    ns = 0
    nblk = 0
    nx = 0
    nm = 0
    for ex in range(ne):
        w2src = w2s[ex].rearrange("(c p) d -> p c d", p=128)
        for dq in range(4):
            for c4 in range(FC // 4):
                j = ns % 2
                ns += 1
                ph.load(stg[j][:, :, :], w2src[:, c4 * 4:(c4 + 1) * 4, dq * 512:(dq + 1) * 512], writes=[tstg[j]])
                ph.pool(lambda e, j=j, c4=c4: e.tensor_copy(w2b[:, c4 * 4:(c4 + 1) * 4, :], stg[j][:, :, :]), reads=[tstg[j]], writes=[tw2b])
            for bi, (b0, w) in enumerate(BLOCKS):
                ai = nblk % 2
                nblk += 1
                ph.load(aT[ai][:, :, 0:w], dr["AT%d" % ex][bi, :, :, 0:w], writes=[taT[ai]])
                for tt in range(w // 128):
                    t0 = b0 + tt * 128
                    ty = 1 if t0 < NCTX else 0
                    q = nm % 4
                    nm += 1
                    for c in range(FC):
                        ph.pe(lambda e, q=q, ai=ai, c=c, tt=tt: e.matmul(pm[q][:, :], aT[ai][:, c, tt * 128:(tt + 1) * 128], w2b[:, c, :],
                                                                         start=(c == 0), stop=(c == FC - 1)),
                              reads=[taT[ai], tw2b], writes=[tpm[q]])
                    xi = nx % 3
                    nx += 1
                    xk = (t0, dq)
                    if xk not in xtok:
                        xtok[xk] = Tok()
                    ph.load(xq[xi][:, :], dr["X"][t0:t0 + 128, dq * 512:(dq + 1) * 512], reads=[xtok[xk]], writes=[txq[xi]])
                    tp = nm % 2
                    ph.dve(lambda e, q=q, tp=tp, ty=ty, dq=dq: e.tensor_tensor(tmp2[tp][:, :], pm[q][:, :], gbc[ty][:, dq * 512:(dq + 1) * 512], ALU.mult),
                           reads=[tpm[q], tg], writes=[ttmp2[tp]])
                    ph.dve(lambda e, xi=xi, tp=tp: e.tensor_tensor(xq[xi][:, :], xq[xi][:, :], tmp2[tp][:, :], ALU.add),
                           reads=[ttmp2[tp], txq[xi]], writes=[txq[xi]])
                    ph.store(dr["X"][t0:t0 + 128, dq * 512:(dq + 1) * 512], xq[xi][:, :], reads=[txq[xi]], writes=[xtok[xk]])
    ph.finish()


Prog.phase_p6b = phase_p6b
```
